# Optimizing a Trainium2 kernel written in Bass

```python
import jax, jax.numpy as jnp
from jax import lax
import numpy as np

D_MODEL = 1024
BATCH = 8
SEQ = 2048
DEPTH = 2

GRID_W = 64
CTX_LEN = 256
HEAD_DIM = 64
ATTN_HEADS = 8
ATTN_WIDTH = ATTN_HEADS * HEAD_DIM
LRU_WIDTH = D_MODEL // 2
LRU_BLOCKS = 8
LRU_BLOCK = LRU_WIDTH // LRU_BLOCKS
LRU_C = 8.0
CONV_W = 4
NA_KH = 8
NA_KW = 16
MIX_WIDTH = ATTN_WIDTH + LRU_WIDTH
IN_COLS = 3 * ATTN_WIDTH + 2 * LRU_WIDTH
SPLITS = (ATTN_WIDTH, 2 * ATTN_WIDTH, 3 * ATTN_WIDTH, 3 * ATTN_WIDTH + LRU_WIDTH)
D_FF = 3584
N_EXPERTS = 8
TOP_K = 2
N_DENSE = (DEPTH + 1) // 2
N_MOE = DEPTH // 2
EPS = 1e-6
NEG_INF = -1e30

kernel_name = 'hybrid_natten_rglru_moe_dit'


def rms_norm(x, g):
    xf = x.astype(jnp.float32)
    y = xf * lax.rsqrt(jnp.mean(xf * xf, axis=-1, keepdims=True) + EPS)
    return (y * g.astype(jnp.float32)).astype(x.dtype)


def modulate(h, shift, scale):
    return h * (1.0 + scale) + shift


def to_heads(t):
    b, s, _ = t.shape
    return t.reshape(b, s, ATTN_HEADS, HEAD_DIM).transpose(0, 2, 1, 3)


def from_heads(t):
    b, h, s, d = t.shape
    return t.transpose(0, 2, 1, 3).reshape(b, s, h * d)


def neighborhood_attention(q, k, v, k_ctx, v_ctx, rpb):
    b, h, s, dh = q.shape
    rows = s // GRID_W
    kh = min(NA_KH, rows)
    kw = NA_KW
    r = jnp.arange(rows)
    col = jnp.arange(GRID_W)
    row_start = jnp.clip(r - kh // 2, 0, rows - kh)
    row_idx = row_start[:, None] + jnp.arange(kh)[None, :]
    col_start = jnp.clip(col - kw // 2, 0, GRID_W - kw)
    col_ok = (col[None, :] >= col_start[:, None]) & (col[None, :] < col_start[:, None] + kw)
    scale = dh ** -0.5
    qg = q.reshape(b, h, rows, GRID_W, dh)
    kg = k.reshape(b, h, rows, GRID_W, dh)[:, :, row_idx]
    vg = v.reshape(b, h, rows, GRID_W, dh)[:, :, row_idx]
    s_win = jnp.einsum('bhrqd,bhrjkd->bhrqjk', qg, kg).astype(jnp.float32) * scale
    dr = row_idx - r[:, None] + (NA_KH - 1)
    dc = jnp.clip(col[None, :] - col[:, None], -(NA_KW - 1), NA_KW - 1) + (NA_KW - 1)
    bias = rpb.astype(jnp.float32)[:, dr[:, None, :, None], dc[None, :, None, :]]
    s_win = jnp.where(col_ok[:, None, :], s_win + bias, NEG_INF)
    s_ctx = jnp.einsum('bhrqd,bhld->bhrql', qg, k_ctx).astype(jnp.float32) * scale
    n_win = kh * GRID_W
    scores = jnp.concatenate([s_win.reshape(b, h, rows, GRID_W, n_win), s_ctx], axis=-1)
    p = jax.nn.softmax(scores, axis=-1).astype(v.dtype)
    p_win = p[..., :n_win].reshape(b, h, rows, GRID_W, kh, GRID_W)
    p_ctx = p[..., n_win:]
    out = jnp.einsum('bhrqjk,bhrjkd->bhrqd', p_win, vg) + jnp.einsum('bhrql,bhld->bhrqd', p_ctx, v_ctx)
    return out.reshape(b, h, s, dh)


def context_attention(q, k, v):
    s = jnp.einsum('bhqd,bhkd->bhqk', q, k).astype(jnp.float32) * (q.shape[-1] ** -0.5)
    p = jax.nn.softmax(s, axis=-1).astype(v.dtype)
    return jnp.einsum('bhqk,bhkd->bhqd', p, v)


def centred_depthwise_conv(x, w, bias):
    t = x.shape[1]
    lo = (CONV_W - 1) // 2
    xp = jnp.pad(x, ((0, 0), (lo, CONV_W - 1 - lo), (0, 0)))
    y = bias + xp[:, 0:t] * w[0]
    for j in range(1, CONV_W):
        y = y + xp[:, j:j + t] * w[j]
    return y


def rglru_coeffs(u, wa, ba, wx, bx, lam):
    b, t, _ = u.shape
    ub = u.reshape(b, t, LRU_BLOCKS, LRU_BLOCK)
    gate_r = jax.nn.sigmoid((jnp.einsum('btni,nij->btnj', ub, wa).reshape(b, t, LRU_WIDTH) + ba).astype(jnp.float32))
    gate_i = jax.nn.sigmoid((jnp.einsum('btni,nij->btnj', ub, wx).reshape(b, t, LRU_WIDTH) + bx).astype(jnp.float32))
    log_a = -LRU_C * gate_r * jax.nn.softplus(-lam.astype(jnp.float32))
    a = jnp.exp(log_a)
    mult = jnp.sqrt(-jnp.expm1(2.0 * log_a))
    return a, mult * gate_i * u.astype(jnp.float32)


def _affine_combine(left, right):
    a_l, b_l = left
    a_r, b_r = right
    return a_l * a_r, a_r * b_l + b_r


def linear_scan(a, b, h0, reverse):
    if h0 is not None:
        edge = -1 if reverse else 0
        b = b.at[:, edge].add(a[:, edge] * h0)
    _, h = lax.associative_scan(_affine_combine, (a, b), reverse=reverse, axis=1)
    return h


def mixing(h_lat, h_ctx, w_in, w_out, rpb, conv_w, conv_b, wa, ba, wx, bx, lam, with_ctx_out):
    q, k, v, xr, gr = jnp.split(h_lat @ w_in, SPLITS, axis=-1)
    qc, kc, vc, xrc, grc = jnp.split(h_ctx @ w_in, SPLITS, axis=-1)
    kc_h, vc_h = to_heads(kc), to_heads(vc)
    o_attn = from_heads(neighborhood_attention(to_heads(q), to_heads(k), to_heads(v), kc_h, vc_h, rpb))
    u_lat = centred_depthwise_conv(xr, conv_w, conv_b)
    u_ctx = centred_depthwise_conv(xrc, conv_w, conv_b)
    y_lat = None
    y_ctx = None
    for d, rev in enumerate((False, True)):
        a_c, b_c = rglru_coeffs(u_ctx, wa[d], ba[d], wx[d], bx[d], lam[d])
        h_c = linear_scan(a_c, b_c, None, rev)
        h_end = h_c[:, 0] if rev else h_c[:, -1]
        a_l, b_l = rglru_coeffs(u_lat, wa[d], ba[d], wx[d], bx[d], lam[d])
        h_l = linear_scan(a_l, b_l, h_end, rev)
        y_lat = h_l if y_lat is None else y_lat + h_l
        if with_ctx_out:
            y_ctx = h_c if y_ctx is None else y_ctx + h_c
    o_lru = jax.nn.gelu(gr) * y_lat.astype(gr.dtype)
    out_lat = jnp.concatenate([o_attn, o_lru], axis=-1) @ w_out
    if not with_ctx_out:
        return out_lat, None
    o_attn_c = from_heads(context_attention(to_heads(qc), kc_h, vc_h))
    o_lru_c = jax.nn.gelu(grc) * y_ctx.astype(grc.dtype)
    out_ctx = jnp.concatenate([o_attn_c, o_lru_c], axis=-1) @ w_out
    return out_lat, out_ctx


def swiglu(h, w1, w3, w2):
    return (jax.nn.silu(h @ w1) * (h @ w3)) @ w2


def moe_swiglu(h, router, router_b, w1, w3, w2):
    logits = (h @ router).astype(jnp.float32) + router_b.astype(jnp.float32)
    top_val, top_idx = lax.top_k(logits, TOP_K)
    wts = jax.nn.softmax(top_val, axis=-1)
    gates = jnp.sum(jax.nn.one_hot(top_idx, N_EXPERTS, dtype=jnp.float32) * wts[..., None], axis=-2).astype(h.dtype)
    y = gates[..., 0:1] * swiglu(h, w1[0], w3[0], w2[0])
    for e in range(1, N_EXPERTS):
        y = y + gates[..., e:e + 1] * swiglu(h, w1[e], w3[e], w2[e])
    return y


def setup_inputs(seed: int = 0) -> dict:
    key = jax.random.key(seed)
    ks = jax.random.split(key, 28)
    f32 = jnp.float32
    D = D_MODEL

    def nrm(k, shape, s):
        return jax.random.normal(k, shape, f32) * s

    u = jax.random.uniform(ks[17], (DEPTH, 2, LRU_WIDTH), f32, 0.9, 0.999)
    a0 = u ** (1.0 / LRU_C)
    lam = jnp.log(a0) - jnp.log1p(-a0)
    return {
        'x': nrm(ks[0], (BATCH, SEQ, D), 1.0),
        'c': nrm(ks[1], (BATCH, D), 1.0),
        'ctx': nrm(ks[2], (BATCH, CTX_LEN, D), 1.0),
        'c_ctx': nrm(ks[3], (D,), 1.0),
        'ada_w': nrm(ks[4], (DEPTH, D, 6 * D), 0.3 * D ** -0.5),
        'ada_b': nrm(ks[5], (DEPTH, 6 * D), 0.02),
        'mix_norm_g': 1.0 + nrm(ks[6], (DEPTH, D), 0.05),
        'ffn_norm_g': 1.0 + nrm(ks[7], (DEPTH, D), 0.05),
        'w_in': nrm(ks[8], (DEPTH, D, IN_COLS), D ** -0.5),
        'w_out': nrm(ks[9], (DEPTH, MIX_WIDTH, D), MIX_WIDTH ** -0.5),
        'na_rpb': nrm(ks[10], (DEPTH, ATTN_HEADS, 2 * NA_KH - 1, 2 * NA_KW - 1), 0.02),
        'conv_w': nrm(ks[11], (DEPTH, CONV_W, LRU_WIDTH), CONV_W ** -0.5),
        'conv_b': nrm(ks[12], (DEPTH, LRU_WIDTH), 0.02),
        'lru_wa': nrm(ks[13], (DEPTH, 2, LRU_BLOCKS, LRU_BLOCK, LRU_BLOCK), LRU_BLOCK ** -0.5),
        'lru_ba': nrm(ks[14], (DEPTH, 2, LRU_WIDTH), 0.02),
        'lru_wx': nrm(ks[15], (DEPTH, 2, LRU_BLOCKS, LRU_BLOCK, LRU_BLOCK), LRU_BLOCK ** -0.5),
        'lru_bx': nrm(ks[16], (DEPTH, 2, LRU_WIDTH), 0.02),
        'lru_lam': lam,
        'ffn_w1': nrm(ks[18], (N_DENSE, D, D_FF), D ** -0.5),
        'ffn_w3': nrm(ks[19], (N_DENSE, D, D_FF), D ** -0.5),
        'ffn_w2': nrm(ks[20], (N_DENSE, D_FF, D), D_FF ** -0.5),
        'moe_router': nrm(ks[21], (N_MOE, D, N_EXPERTS), D ** -0.5),
        'moe_router_b': nrm(ks[22], (N_MOE, N_EXPERTS), 0.01),
        'moe_w1': nrm(ks[23], (N_MOE, N_EXPERTS, D, D_FF), D ** -0.5),
        'moe_w3': nrm(ks[24], (N_MOE, N_EXPERTS, D, D_FF), D ** -0.5),
        'moe_w2': nrm(ks[25], (N_MOE, N_EXPERTS, D_FF, D), D_FF ** -0.5),
        'final_g': 1.0 + nrm(ks[26], (D,), 0.05),
    }


def reference(x, c, ctx, c_ctx, ada_w, ada_b, mix_norm_g, ffn_norm_g, w_in, w_out, na_rpb, conv_w, conv_b, lru_wa, lru_ba, lru_wx, lru_bx, lru_lam, ffn_w1, ffn_w3, ffn_w2, moe_router, moe_router_b, moe_w1, moe_w3, moe_w2, final_g):
    silu_c = jax.nn.silu(c)[:, None, :]
    silu_cc = jax.nn.silu(c_ctx)[None, None, :]
    h_lat, h_ctx = x, ctx
    for i in range(DEPTH):
        last = i == DEPTH - 1
        sh1, sc1, g1, sh2, sc2, g2 = jnp.split(silu_c @ ada_w[i] + ada_b[i], 6, axis=-1)
        csh1, csc1, cg1, csh2, csc2, cg2 = jnp.split(silu_cc @ ada_w[i] + ada_b[i], 6, axis=-1)
        a_lat = modulate(rms_norm(h_lat, mix_norm_g[i]), sh1, sc1)
        a_ctx = modulate(rms_norm(h_ctx, mix_norm_g[i]), csh1, csc1)
        o_lat, o_ctx = mixing(a_lat, a_ctx, w_in[i], w_out[i], na_rpb[i], conv_w[i], conv_b[i],
                              lru_wa[i], lru_ba[i], lru_wx[i], lru_bx[i], lru_lam[i], not last)
        h_lat = h_lat + g1 * o_lat
        if i % 2 == 0:
            j = i // 2
            ffn = lambda t, j=j: swiglu(t, ffn_w1[j], ffn_w3[j], ffn_w2[j])
        else:
            j = i // 2
            ffn = lambda t, j=j: moe_swiglu(t, moe_router[j], moe_router_b[j], moe_w1[j], moe_w3[j], moe_w2[j])
        h_lat = h_lat + g2 * ffn(modulate(rms_norm(h_lat, ffn_norm_g[i]), sh2, sc2))
        if not last:
            h_ctx = h_ctx + cg1 * o_ctx
            h_ctx = h_ctx + cg2 * ffn(modulate(rms_norm(h_ctx, ffn_norm_g[i]), csh2, csc2))
    return rms_norm(h_lat, final_g)
```

```python
import numpy as np
from contextlib import ExitStack
import concourse.bass as bass
import concourse.mybir as mybir
from concourse.bass_utils import run_bass_kernel_spmd

F32 = mybir.dt.float32
BF16 = mybir.dt.bfloat16
AF = mybir.ActivationFunctionType
ALU = mybir.AluOpType

ENGINES = ('pe', 'act', 'dve', 'pool', 'sp')


class Rec:
    def __init__(self):
        self.ops = {e: [] for e in ENGINES}
        self.cnt = {}
        self.last_w = {}
        self.readers = {}
        self.waited = {e: {} for e in ENGINES}
        self.region = None
        self.pre_counts = {}
        self.regions = []
        self._snap = None

    def begin_region(self, flag_ap, flag_res):
        for eng in ENGINES:
            self.op(eng, [], reads=[flag_res], final=True)
        self.regions.append(flag_ap)
        self.region = len(self.regions) - 1
        self._snap = {e: dict(w) for e, w in self.waited.items()}

    def end_region(self):
        self.region = None
        self.waited = self._snap
        self._snap = None

    def op(self, eng, fns, reads=(), writes=(), dma=None, final=False):
        deps = {}

        def need(cv):
            c, v = cv
            if deps.get(c, 0) < v:
                deps[c] = v

        for r in reads:
            if r in self.last_w:
                need(self.last_w[r])
        for w in writes:
            if w in self.last_w:
                need(self.last_w[w])
            for cv in self.readers.get(w, {}).items():
                need(cv)
        waits = []
        for c, v in deps.items():
            if c == 'pe' and eng == 'pe' and dma is None:
                continue
            if self.waited[eng].get(c, 0) >= v:
                continue
            self.waited[eng][c] = v
            waits.append((c, v))
        if final:
            self.ops[eng].append((waits, [], None, 0, self.region))
            return
        ctr = ('dma:' + dma) if dma else eng
        step = 16 if dma else 1
        if self.region is not None:
            d_ = self.pre_counts.setdefault((eng, self.region), {})
            if ctr not in d_:
                d_[ctr] = self.cnt.get(ctr, 0)
        val = self.cnt.get(ctr, 0) + step
        self.cnt[ctr] = val
        self.ops[eng].append((waits, list(fns), ctr, step, self.region))
        for r in reads:
            self.readers.setdefault(r, {})[ctr] = val
        for w in writes:
            self.last_w[w] = (ctr, val)
            self.readers[w] = {}

    def barrier(self):
        for eng in ENGINES:
            waits = []
            for c, v in self.cnt.items():
                if self.waited[eng].get(c, 0) >= v:
                    continue
                self.waited[eng][c] = v
                waits.append((c, v))
            self.ops[eng].append((waits, [], None, 0, None))
        self.last_w = {}
        self.readers = {}


def emit(nc, rec, es):
    names = sorted(rec.cnt.keys())
    sems = {n: es.enter_context(nc.semaphore(n.replace(':', '_'))) for n in names}
    block = es.enter_context(nc.Block())

    def run_ops(e, ops):
        for waits, fns, ctr, step, _ in ops:
            for c, v in waits:
                e.wait_ge(sems[c], v)
            ins = None
            for fn in fns:
                ins = fn(e)
            if ins is not None and ctr is not None:
                ins.then_inc(sems[ctr], step)

    def replay(e, eng):
        ops = rec.ops[eng]
        if not rec.regions:
            run_ops(e, ops)
            return
        with e.register(f"flag_{eng}") as reg:
            i = 0
            while i < len(ops):
                rg = ops[i][4]
                j = i
                while j < len(ops) and ops[j][4] == rg:
                    j += 1
                run = ops[i:j]
                i = j
                if rg is None:
                    run_ops(e, run)
                    continue
                tot, pre = {}, {}
                cur = {}
                for waits, fns, ctr, step, _ in run:
                    if ctr is None:
                        continue
                    tot[ctr] = tot.get(ctr, 0) + step
                if not tot:
                    continue
                for c in tot:
                    pre[c] = rec.pre_counts[(eng, rg)][c]
                e.reg_load(reg, rec.regions[rg])
                with e.If_ne(reg, 0):
                    run_ops(e, run)
                with e.Else():
                    for c in tot:
                        e.wait_ge(sems[c], pre[c])
                        e.sem_inc(sems[c], tot[c])

    @block.tensor
    def _(e):
        replay(e, 'pe')

    @block.scalar
    def _(e):
        replay(e, 'act')

    @block.vector
    def _(e):
        replay(e, 'dve')

    @block.gpsimd
    def _(e):
        replay(e, 'pool')

    @block.sync
    def _(e):
        replay(e, 'sp')


NT = 2304
NL = 2048
BLKS = [(0, 512), (512, 512), (1024, 512), (1536, 512), (2048, 256)]
EPS = 1e-6
DFF = 3584
NEG = -200.0
LROWS = 108
R_ADAB, R_MNG, R_FNG, R_CONVW, R_CONVB, R_BA, R_BX, R_LAM = 0, 48, 56, 64, 80, 84, 92, 100
R_FINALG, R_C, R_CC = 216, 224, 232
SCN = 15000
import os
ATT_PARTIAL = 0
MOE_SPARSE = int(os.environ.get('MOE_SPARSE', '1'))
I32 = mybir.dt.int32


def mkap(base, dims):
    return bass.AP(base.tensor, base.offset, [list(base.ap[0])] + [list(d) for d in dims])


def build_program(debug=None):
    nc = bass.Bass("TRN2", target_bir_lowering=False)

    def din(name, shape):
        return nc.dram_tensor(name, shape, F32, kind="ExternalInput").ap()

    x_d = din("x", [2048, 1024])
    ctx_d = din("ctx", [256, 1024])
    vecs_d = din("vecs", [256, 128])
    ada_w = din("ada_w", [2, 1024, 6144])
    w_in = din("w_in", [2, 1024, 2560])
    w_out = din("w_out", [2, 1024, 1024])
    ffn_w1 = din("ffn_w1", [1, 1024, DFF])
    ffn_w3 = din("ffn_w3", [1, 1024, DFF])
    ffn_w2 = din("ffn_w2", [1, DFF, 1024])
    moe_w1 = din("moe_w1", [1, 8, 1024, DFF])
    moe_w3 = din("moe_w3", [1, 8, 1024, DFF])
    moe_w2 = din("moe_w2", [1, 8, DFF, 1024])
    router_d = din("moe_router", [1, 1024, 8])
    rb_d = din("rb", [8, 1])
    wbd_d = din("wbd", [2, 2, 2, 4, 128, 128])
    tb_d = din("tb", [2, 4, 2, 2, 128, 1024])
    ident_d = din("ident", [128, 128])
    sel_d = din("sel", [8, 1024])
    ustr_d = din("ustrict", [128, 128])
    iota_d = din("iota", [128, 512])
    out_d = nc.dram_tensor("out", [2048, 1024], F32, kind="ExternalOutput").ap()
    dbg_d = None
    if debug is not None:
        dbg_d = nc.dram_tensor("dbg", [128, 8 * NT], F32, kind="ExternalOutput").ap()

    es = ExitStack()
    with es:
        def sb(name, shape, dt=F32):
            return es.enter_context(nc.sbuf_tensor('s_' + name, shape, dt))

        hT = sb("hT", [128, 8, NT])
        aT = sb("aT", [128, 8, NT], BF16)
        qo = sb("qo", [128, 4, NT], BF16)
        gro = sb("gro", [128, 4, NT], BF16)
        vT = sb("vT", [128, 256])
        ident = sb("ident", [128, 128])
        onesf = sb("onesf", [128, 128])
        cst = sb("cst", [128, 16])
        identb = sb("identb", [128, 128], BF16)
        flags_i = sb("flags_i", [128, 32], I32)
        mod = sb("mod", [128, 2, 48, 2])
        gsc = sb("gsc", [128, 2, 2, 8, 2])
        scb = sb("scb", [128, 8, 2], BF16)
        lruc = sb("lruc", [128, 2, 3, 8])
        router_sb = sb("router_sb", [128, 8, 8])
        rb_sb = sb("rb_sb", [8, 1])
        selb = [sb("selb0", [8, 128]), sb("selb1", [8, 128])]
        sc = sb("sc", [128, SCN])
        ps = es.enter_context(nc.psum_tensor("ps", [128, 4096], F32))

        def bank(i, n=512, off=0):
            return ps[:, i * 512 + off:i * 512 + off + n]

        def PB(i):
            return f"ps{i}"

        r = Rec()
        ctr = {'cp': 0}

        class Carver:
            def __init__(self):
                self.off = 0

            def f32(self, n):
                v = sc[:, self.off:self.off + n]
                self.off += n
                assert self.off <= SCN, self.off
                return v

            def bf16(self, n):
                assert n % 2 == 0
                v = sc[:, self.off:self.off + n // 2].bitcast(BF16)
                self.off += n // 2
                assert self.off <= SCN, self.off
                return v

        def copy_any(out, in_, reads, writes, eng=None):
            if eng is None:
                eng = 'act' if ctr['cp'] % 2 == 0 else 'dve'
                ctr['cp'] += 1
            if eng == 'act':
                r.op('act', [lambda e: e.activation(out=out, in_=in_, func=AF.Identity)], reads=reads, writes=writes)
            else:
                r.op(eng, [lambda e: e.tensor_copy(out=out, in_=in_)], reads=reads, writes=writes)

        qof_ = qo[:].rearrange("p c t -> p (c t)")
        grf_ = gro[:].rearrange("p c t -> p (c t)")

        def a2tok(t):
            return qof_[:, t * 1024:(t + 1) * 1024] if t < 9 else grf_[:, (t - 9) * 1024:(t - 8) * 1024]

        def vcol(row):
            return vT[:, row:row + 1]

        def blk_of(t):
            return min(t // 512, 4)

        cv = Carver()
        vst = cv.f32(256).rearrange("p (g n) -> p g n", g=2)
        xst = [cv.f32(1024) for _ in range(4)]
        r.op('sp', [lambda e: e.dma_start(out=ident[:], in_=ident_d)], writes=['ident'], dma='c0')
        r.op('sp', [lambda e: e.dma_start(out=vst, in_=vecs_d.rearrange("(g p) n -> p g n", p=128))], writes=['vst'], dma='c1')
        r.op('sp', [lambda e: e.dma_start(out=router_sb[:], in_=router_d[0].rearrange("(c p) n -> p c n", p=128))], writes=['router'], dma='c2')
        r.op('sp', [lambda e: e.dma_start(out=rb_sb[:], in_=rb_d)], writes=['rb'], dma='c3')
        r.op('pool', [lambda e: e.memset(onesf[:], 1.0 / 1024.0)], writes=['onesf'])
        r.op('pool', [lambda e: e.memset(cst[:, 0:1], EPS)], writes=['cst'])
        r.op('pool', [lambda e: e.memset(cst[:, 1:2], 1.0)], writes=['cst'])
        r.op('pool', [lambda e: e.memset(cst[:, 2:3], -1e30)], writes=['cst'])
        r.op('pool', [lambda e: e.memset(cst[:, 3:4], 0.0)], writes=['cst'])
        r.op('pool', [lambda e: e.memset(cst[:, 4:5], -1.0)], writes=['cst'])
        for b_ in range(4):
            r.op('pool', [lambda e, b_=b_: e.memset(cst[:, 5 + b_:6 + b_], 512.0 * b_ + 0.5)], writes=['cst'])
            r.op('pool', [lambda e, b_=b_: e.memset(cst[:, 9 + b_:10 + b_], -512.0 * b_)], writes=['cst'])
        r.op('dve', [lambda e: e.tensor_copy(out=identb[:], in_=ident[:])], reads=['ident'], writes=['identb'])
        for g in range(2):
            r.op('pe', [lambda e, g=g: e.transpose(out=bank(0, 128, g * 128), in_=vst[:, g, :], identity=ident[:])],
                 reads=['vst', 'ident'], writes=[PB(0)])
        r.op('dve', [lambda e: e.tensor_copy(out=vT[:], in_=bank(0, 256))], writes=[PB(0), 'vT'])
        r.op('act', [lambda e: e.activation(out=scb[:, :, 0], in_=vT[:, R_C:R_C + 8], func=AF.Silu)], reads=['vT'], writes=['scb0'])
        r.op('act', [lambda e: e.activation(out=scb[:, :, 1], in_=vT[:, R_CC:R_CC + 8], func=AF.Silu)], reads=['vT'], writes=['scb1'])
        for l in range(2):
            lam = vT[:, l * LROWS + R_LAM:l * LROWS + R_LAM + 8]
            r.op('act', [lambda e, l=l, lam=lam: e.activation(out=lruc[:, l, 2, :], in_=lam, func=AF.Exp, scale=-1.0)], reads=['vT'], writes=[f'lruc{l}'])
            r.op('act', [lambda e, l=l: e.activation(out=lruc[:, l, 2, :], in_=lruc[:, l, 2, :], func=AF.Ln, bias=cst[:, 1:2])], reads=['cst'], writes=[f'lruc{l}'])
            r.op('act', [lambda e, l=l: e.activation(out=lruc[:, l, 0, :], in_=lruc[:, l, 2, :], func=AF.Identity, scale=-8.0)], writes=[f'lruc{l}'])
            r.op('act', [lambda e, l=l: e.activation(out=lruc[:, l, 1, :], in_=lruc[:, l, 2, :], func=AF.Identity, scale=-16.0)], writes=[f'lruc{l}'])

        for tile in range(18):
            src = x_d[tile * 128:(tile + 1) * 128, :] if tile < 16 else ctx_d[(tile - 16) * 128:(tile - 15) * 128, :]
            xs = xst[tile % 4]
            r.op('sp', [lambda e, xs=xs, src=src: e.dma_start(out=xs, in_=src)], writes=[f'xst{tile % 4}'], dma=f'x{tile % 4}')
            bk = (tile % 2) * 2 + 2
            for half in range(2):
                r.op('pe', [lambda e, xs=xs, c=c, bk=bk, half=half: e.transpose(
                    out=bank(bk + half, 128, (c % 4) * 128), in_=xs[:, c * 128:(c + 1) * 128], identity=ident[:])
                    for c in range(half * 4, half * 4 + 4)],
                    reads=[f'xst{tile % 4}', 'ident'], writes=[PB(bk + half)])
                dst = hT[:, half * 4:half * 4 + 4, tile * 128:(tile + 1) * 128]
                srcp = bank(bk + half).rearrange("p (c n) -> p c n", c=4)
                copy_any(dst, srcp, [], [PB(bk + half)] + [f'hT:{c}:{blk_of(tile * 128)}' for c in range(half * 4, half * 4 + 4)])

        def dump_hT_and_finish():
            for c in range(8):
                r.op('sp', [lambda e, c=c: e.dma_start(out=dbg_d[:, c * NT:(c + 1) * NT], in_=hT[:, c, :])],
                     reads=[f'hT:{c}:{b}' for b in range(5)], writes=['dbg'], dma='dbg')
            r.op('sp', [], reads=['dbg'], final=True)

        def dump_bf16(t3, nchunks, resnames):
            r.barrier()
            cvd = Carver()
            stg = cvd.f32(NT)
            for c in range(nchunks):
                r.op('dve', [lambda e, c=c: e.tensor_copy(out=stg, in_=t3[:, c, :])], reads=resnames(c), writes=['stg'])
                r.op('sp', [lambda e, c=c: e.dma_start(out=dbg_d[:, c * NT:(c + 1) * NT], in_=stg)], reads=['stg'], writes=['dbg'], dma='dbg')
            r.op('sp', [], reads=['dbg'], final=True)

        def adaln(l):
            cva = Carver()
            cva.off = 4352
            wA = [cva.bf16(8 * 1024).rearrange("p (k n) -> p k n", k=8) for _ in range(2)]

            def load(g):
                buf = wA[g % 2]
                r.op('pool', [lambda e: e.dma_start(out=buf, in_=ada_w[l][:, g * 1024:(g + 1) * 1024].rearrange("(k p) n -> p k n", p=128))],
                     writes=[f'wA{g % 2}'], dma=f'wA{g % 2}')

            load(0)
            for g in range(6):
                if g + 1 < 6:
                    load(g + 1)
                buf = wA[g % 2]
                pbk = g % 2
                for j in range(8):
                    r.op('pe', [lambda e, buf=buf, j=j, k=k, pbk=pbk: e.matmul(bank(pbk, 2, j * 2), buf[:, k, j * 128:(j + 1) * 128], scb[:, k, :],
                                                                       start=(k == 0), stop=(k == 7)) for k in range(8)],
                         reads=[f'wA{g % 2}', 'scb0', 'scb1'], writes=[PB(pbk)])
                psv = bank(pbk, 16).rearrange("p (j t) -> p j t", t=2)
                for col in range(2):
                    r.op('dve', [lambda e, psv=psv, col=col, g=g: e.tensor_tensor(
                        out=mod[:, l, g * 8:(g + 1) * 8, col], in0=psv[:, :, col],
                        in1=vT[:, l * LROWS + R_ADAB + g * 8:l * LROWS + R_ADAB + g * 8 + 8], op=ALU.add)],
                        reads=['vT'], writes=[PB(pbk), f'mod{l}'])
            for n, (grow, scw) in enumerate(((R_MNG, 1), (R_FNG, 4))):
                for col in range(2):
                    r.op('dve', [lambda e, n=n, grow=grow, scw=scw, col=col: e.scalar_tensor_tensor(
                        out=gsc[:, l, n, :, col], in0=mod[:, l, scw * 8:scw * 8 + 8, col], scalar=cst[:, 1:2],
                        in1=vT[:, l * LROWS + grow:l * LROWS + grow + 8], op0=ALU.add, op1=ALU.mult)],
                        reads=[f'mod{l}', 'vT', 'cst'], writes=[f'gsc{l}'])

        def norm_mod(l, n, blocks, cvx, router=False, logitsT=None):
            sq = [cvx.f32(512) for _ in range(2)]
            lnv = cvx.f32(512)
            rstd = [cvx.f32(512) for _ in range(2)]
            tm = [cvx.f32(512) for _ in range(2)]
            a2f = [cvx.f32(512) for _ in range(2)] if router else None
            shw = 0 if n == 0 else 3
            for bi in blocks:
                t0, sz = BLKS[bi]
                col = 1 if bi == 4 else 0
                pbk = bi % 2
                for c in range(8):
                    s = sq[c % 2]
                    r.op('act', [lambda e, s=s, c=c, t0=t0, sz=sz: e.activation(out=s[:, :sz], in_=hT[:, c, t0:t0 + sz], func=AF.Square)],
                         reads=[f'hT:{c}:{bi}'], writes=[f'sq{c % 2}'])
                    r.op('pe', [lambda e, s=s, c=c, pbk=pbk, sz=sz: e.matmul(bank(pbk, sz), onesf[:], s[:, :sz], start=(c == 0), stop=(c == 7))],
                         reads=[f'sq{c % 2}', 'onesf'], writes=[PB(pbk)])
                rs = rstd[bi % 2]
                r.op('act', [lambda e, pbk=pbk, sz=sz: e.activation(out=lnv[:, :sz], in_=bank(pbk, sz), func=AF.Ln, bias=cst[:, 0:1])],
                     reads=['cst'], writes=[PB(pbk), 'lnv'])
                r.op('act', [lambda e, rs=rs, sz=sz: e.activation(out=rs[:, :sz], in_=lnv[:, :sz], func=AF.Exp, scale=-0.5)],
                     reads=['lnv'], writes=[f'rstd{bi % 2}'])
                for c in range(8):
                    t = tm[c % 2]
                    r.op('dve', [lambda e, t=t, c=c, rs=rs, t0=t0, sz=sz: e.tensor_tensor(out=t[:, :sz], in0=hT[:, c, t0:t0 + sz], in1=rs[:, :sz], op=ALU.mult)],
                         reads=[f'hT:{c}:{bi}', f'rstd{bi % 2}'], writes=[f'tm{c % 2}'])
                    bias_ap = mod[:, l, shw * 8 + c, col:col + 1]
                    scale_ap = gsc[:, l, n, c, col:col + 1]
                    if not router:
                        r.op('act', [lambda e, t=t, c=c, t0=t0, sz=sz, bias_ap=bias_ap, scale_ap=scale_ap: e.activation(
                            out=aT[:, c, t0:t0 + sz], in_=t[:, :sz], func=AF.Identity, bias=bias_ap, scale=scale_ap)],
                            reads=[f'tm{c % 2}', f'mod{l}', f'gsc{l}'], writes=[f'aT:{c}:{bi}'])
                    else:
                        af = a2f[c % 2]
                        r.op('act', [lambda e, t=t, af=af, sz=sz, bias_ap=bias_ap, scale_ap=scale_ap: e.activation(
                            out=af[:, :sz], in_=t[:, :sz], func=AF.Identity, bias=bias_ap, scale=scale_ap)],
                            reads=[f'tm{c % 2}', f'mod{l}', f'gsc{l}'], writes=[f'a2f{c % 2}'])
                        if not MOE_SPARSE:
                            r.op('dve', [lambda e, c=c, af=af, t0=t0, sz=sz: e.tensor_copy(out=aT[:, c, t0:t0 + sz], in_=af[:, :sz])],
                                 reads=[f'a2f{c % 2}'], writes=[f'aT:{c}:{bi}'])
                        else:
                            pbt = 4 + c % 2
                            r.op('pe', [lambda e, af=af, tt=tt, pbt=pbt: e.transpose(out=bank(pbt, 128, tt * 128), in_=af[:, tt * 128:(tt + 1) * 128], identity=ident[:])
                                        for tt in range(sz // 128)], reads=[f'a2f{c % 2}', 'ident'], writes=[PB(pbt)])
                            for tt in range(sz // 128):
                                tk = t0 // 128 + tt
                                copy_any(a2tok(tk)[:, c * 128:(c + 1) * 128], bank(pbt, 128, tt * 128), [], [PB(pbt), f'a2tok{tk}'])
                        r.op('pe', [lambda e, c=c, af=af, bi=bi, sz=sz: e.matmul(bank(2 + bi % 2, sz)[0:8, :], router_sb[:, c, :], af[:, :sz], start=(c == 0), stop=(c == 7))],
                             reads=[f'a2f{c % 2}', 'router'], writes=[PB(2 + bi % 2)])
                if router:
                    r.op('act', [lambda e, bi=bi, t0=t0, sz=sz: e.activation(out=logitsT[0:8, t0:t0 + sz], in_=bank(2 + bi % 2, sz)[0:8, :],
                                                                           func=AF.Identity, bias=rb_sb[0:8, 0:1])],
                         reads=['rb'], writes=[PB(2 + bi % 2), 'logitsT'])

        def lru_phase(l, last):
            cvl = Carver()
            wx = [cvl.bf16(8 * 128).rearrange("p (k n) -> p k n", k=8) for _ in range(2)]
            wg = [cvl.bf16(8 * 128).rearrange("p (k n) -> p k n", k=8) for _ in range(2)]
            wb = [cvl.bf16(4 * 128).rearrange("p (d a n) -> p d a n", d=2, a=2) for _ in range(2)]
            xp = cvl.f32(2312)
            u = cvl.f32(NT)
            ub = cvl.bf16(NT)
            tr_ = [cvl.f32(512) for _ in range(3)]
            ti_ = [cvl.f32(512) for _ in range(3)]
            ta_ = [cvl.f32(512) for _ in range(2)]
            tm_ = [cvl.f32(512) for _ in range(2)]
            thr = [cvl.f32(512) for _ in range(2)]
            hf = xp[:, 0:NT]
            base = l * LROWS

            def load(c):
                b = c % 2
                r.op('pool', [lambda e: e.dma_start(out=wx[b], in_=w_in[l][:, 1536 + c * 128:1536 + (c + 1) * 128].rearrange("(k p) n -> p k n", p=128))],
                     writes=[f'wx{b}'], dma=f'wx{b}')
                r.op('pool', [lambda e: e.dma_start(out=wg[b], in_=w_in[l][:, 2048 + c * 128:2048 + (c + 1) * 128].rearrange("(k p) n -> p k n", p=128))],
                     writes=[f'wg{b}'], dma=f'wg{b}')
                r.op('pool', [lambda e: e.dma_start(out=wb[b], in_=wbd_d[l, :, :, c].rearrange("d a p n -> p d a n"))],
                     writes=[f'wb{b}'], dma=f'wb{b}')

            load(0)
            it = 0
            for c in range(4):
                if c + 1 < 4:
                    load(c + 1)
                b = c % 2
                for (a0, a1) in ((0, 1), (2049, 2052), (2308, 2312)):
                    r.op('pool', [lambda e, a0=a0, a1=a1: e.memset(xp[:, a0:a1], 0.0)], writes=['xp'])
                for bi in range(5):
                    t0, sz = BLKS[bi]
                    xo = t0 + 1 if bi < 4 else t0 + 4
                    pa, pg = 4 + bi % 2, 6 + bi % 2
                    r.op('pe', [lambda e, k=k, pa=pa, b=b, t0=t0, sz=sz: e.matmul(bank(pa, sz), wx[b][:, k, :], aT[:, k, t0:t0 + sz], start=(k == 0), stop=(k == 7)) for k in range(8)],
                         reads=[f'wx{b}'] + [f'aT:{k}:{bi}' for k in range(8)], writes=[PB(pa)])
                    r.op('act', [lambda e, pa=pa, xo=xo, sz=sz: e.activation(out=xp[:, xo:xo + sz], in_=bank(pa, sz), func=AF.Identity)],
                         writes=[PB(pa), 'xp'])
                    r.op('pe', [lambda e, k=k, pg=pg, b=b, t0=t0, sz=sz: e.matmul(bank(pg, sz), wg[b][:, k, :], aT[:, k, t0:t0 + sz], start=(k == 0), stop=(k == 7)) for k in range(8)],
                         reads=[f'wg{b}'] + [f'aT:{k}:{bi}' for k in range(8)], writes=[PB(pg)])
                    r.op('act', [lambda e, pg=pg, c=c, t0=t0, sz=sz: e.activation(out=gro[:, c, t0:t0 + sz], in_=bank(pg, sz), func=AF.Gelu_apprx_tanh)],
                         writes=[PB(pg), f'gro:{c}:{bi}'])
                for (o0, n0, x0) in ((0, NL, 0), (NL, 256, 2051)):
                    r.op('dve', [lambda e, o0=o0, n0=n0, x0=x0, c=c: e.tensor_scalar(
                        out=u[:, o0:o0 + n0], in0=xp[:, x0:x0 + n0], scalar1=vcol(base + R_CONVW + 0 * 4 + c), scalar2=vcol(base + R_CONVB + c),
                        op0=ALU.mult, op1=ALU.add)], reads=['xp', 'vT'], writes=['u'])
                    for j in range(1, 4):
                        r.op('dve', [lambda e, o0=o0, n0=n0, x0=x0, j=j, c=c: e.scalar_tensor_tensor(
                            out=u[:, o0:o0 + n0], in0=xp[:, x0 + j:x0 + j + n0], scalar=vcol(base + R_CONVW + j * 4 + c), in1=u[:, o0:o0 + n0],
                            op0=ALU.mult, op1=ALU.add)], reads=['xp', 'vT'], writes=['u'])
                r.op('act', [lambda e: e.activation(out=ub, in_=u, func=AF.Identity)], reads=['u'], writes=['ub'])
                for d in range(2):
                    order = [4, 0, 1, 2, 3] if d == 0 else [4, 3, 2, 1, 0]
                    prev = None
                    prev_k2 = None
                    ba_ap = vcol(base + R_BA + d * 4 + c)
                    bx_ap = vcol(base + R_BX + d * 4 + c)
                    nsp1 = lruc[:, l, 0, d * 4 + c:d * 4 + c + 1]
                    nsp2 = lruc[:, l, 1, d * 4 + c:d * 4 + c + 1]
                    for grp in (order[0:2], order[2:4], order[4:5]):
                        infos = []
                        for bi in grp:
                            t0, sz = BLKS[bi]
                            k2 = it % 2
                            k3 = it % 3
                            it += 1
                            pr, pi = 4 + k2, 6 + k2
                            tr, ti = tr_[k3], ti_[k3]
                            r.op('pe', [lambda e, pr=pr, b=b, d=d, t0=t0, sz=sz: e.matmul(bank(pr, sz), wb[b][:, d, 0, :], ub[:, t0:t0 + sz], start=True, stop=True)],
                                 reads=[f'wb{b}', 'ub'], writes=[PB(pr)])
                            r.op('pe', [lambda e, pi=pi, b=b, d=d, t0=t0, sz=sz: e.matmul(bank(pi, sz), wb[b][:, d, 1, :], ub[:, t0:t0 + sz], start=True, stop=True)],
                                 reads=[f'wb{b}', 'ub'], writes=[PB(pi)])
                            r.op('act', [lambda e, pr=pr, tr=tr, sz=sz, ba_ap=ba_ap: e.activation(out=tr[:, :sz], in_=bank(pr, sz), func=AF.Sigmoid, bias=ba_ap)],
                                 reads=['vT'], writes=[PB(pr), f'tr{k3}'])
                            r.op('act', [lambda e, pi=pi, ti=ti, sz=sz, bx_ap=bx_ap: e.activation(out=ti[:, :sz], in_=bank(pi, sz), func=AF.Sigmoid, bias=bx_ap)],
                                 reads=['vT'], writes=[PB(pi), f'ti{k3}'])
                            infos.append((bi, t0, sz, k2, k3))
                        for (bi, t0, sz, k2, k3) in infos:
                            tr, ta, tmm = tr_[k3], ta_[k2], tm_[k2]
                            r.op('act', [lambda e, tr=tr, ta=ta, sz=sz, nsp1=nsp1: e.activation(out=ta[:, :sz], in_=tr[:, :sz], func=AF.Exp, scale=nsp1)],
                                 reads=[f'tr{k3}', f'lruc{l}'], writes=[f'ta{k2}'])
                            r.op('act', [lambda e, tr=tr, tmm=tmm, sz=sz, nsp2=nsp2: e.activation(out=tmm[:, :sz], in_=tr[:, :sz], func=AF.Exp, scale=nsp2)],
                                 reads=[f'tr{k3}', f'lruc{l}'], writes=[f'tm{k2}'])
                            r.op('act', [lambda e, tmm=tmm, sz=sz: e.activation(out=tmm[:, :sz], in_=tmm[:, :sz], func=AF.Ln, scale=-1.0, bias=cst[:, 1:2])],
                                 reads=['cst'], writes=[f'tm{k2}'])
                            r.op('act', [lambda e, tmm=tmm, sz=sz: e.activation(out=tmm[:, :sz], in_=tmm[:, :sz], func=AF.Exp, scale=0.5)],
                                 writes=[f'tm{k2}'])
                        for (bi, t0, sz, k2, k3) in infos:
                            ti, ta, tmm, hr = ti_[k3], ta_[k2], tm_[k2], thr[k2]
                            r.op('dve', [lambda e, ti=ti, tmm=tmm, sz=sz: e.tensor_tensor(out=ti[:, :sz], in0=ti[:, :sz], in1=tmm[:, :sz], op=ALU.mult)],
                                 reads=[f'tm{k2}'], writes=[f'ti{k3}'])
                            r.op('dve', [lambda e, ti=ti, t0=t0, sz=sz: e.tensor_tensor(out=ti[:, :sz], in0=ti[:, :sz], in1=u[:, t0:t0 + sz], op=ALU.mult)],
                                 reads=['u'], writes=[f'ti{k3}'])
                            if d == 0:
                                if prev is None:
                                    init = 0.0
                                else:
                                    pt0, psz = BLKS[prev]
                                    init = hf[:, pt0 + psz - 1:pt0 + psz]
                                r.op('dve', [lambda e, ta=ta, ti=ti, init=init, t0=t0, sz=sz: e.tensor_tensor_scan(
                                    out=hf[:, t0:t0 + sz], data0=ta[:, :sz], data1=ti[:, :sz], initial=init, op0=ALU.mult, op1=ALU.add)],
                                    reads=[f'ta{k2}', f'ti{k3}'], writes=['xp'])
                            else:
                                init = 0.0 if prev is None else thr[prev_k2][:, 0:1]
                                rd = [f'ta{k2}', f'ti{k3}'] + ([] if prev is None else [f'hr{prev_k2}'])
                                r.op('dve', [lambda e, ta=ta, ti=ti, hr=hr, init=init, sz=sz: e.tensor_tensor_scan(
                                    out=mkap(hr[:, sz - 1:sz], [[-1, sz]]), data0=mkap(ta[:, sz - 1:sz], [[-1, sz]]),
                                    data1=mkap(ti[:, sz - 1:sz], [[-1, sz]]), initial=init, op0=ALU.mult, op1=ALU.add)],
                                    reads=rd, writes=[f'hr{k2}'])
                                if not (last and bi == 4):
                                    r.op('dve', [lambda e, hr=hr, tmm=tmm, t0=t0, sz=sz: e.tensor_tensor(out=tmm[:, :sz], in0=hr[:, :sz], in1=hf[:, t0:t0 + sz], op=ALU.add)],
                                         reads=[f'hr{k2}', 'xp'], writes=[f'tm{k2}'])
                                    r.op('dve', [lambda e, tmm=tmm, c=c, t0=t0, sz=sz: e.tensor_tensor(out=gro[:, c, t0:t0 + sz], in0=gro[:, c, t0:t0 + sz], in1=tmm[:, :sz], op=ALU.mult)],
                                         reads=[f'tm{k2}'], writes=[f'gro:{c}:{bi}'])
                            prev = bi
                            prev_k2 = k2

        def attn_phase(l, last):
            cva = Carver()
            wq = [cva.bf16(1024).rearrange("p (k n) -> p k n", k=8)] * 2
            wk = [cva.bf16(1024).rearrange("p (k n) -> p k n", k=8)] * 2
            wv = [cva.bf16(1024).rearrange("p (k n) -> p k n", k=8)] * 2
            kT = [cva.bf16(NT) for _ in range(2)]
            V = [cva.bf16(18 * 192).rearrange("p (t n) -> p t n", t=18) for _ in range(2)]
            T = [cva.bf16(4096).rearrange("p (v j n) -> p v j n", v=2, j=2) for _ in range(2)]
            Tb32 = [cva.f32(1024) for _ in range(2)]
            P = [cva.bf16(1024) for _ in range(2)]
            rec = [cva.f32(128) for _ in range(2)]
            otok = [cva.bf16(128) for _ in range(2)]
            for b in range(2):
                r.op('pool', [lambda e, b=b: e.memset(V[b][:, :, 64:128], 1.0)], writes=[f'V{b}'])

            def load(hp):
                for nm, buf, c0 in (('wq', wq, 0), ('wk', wk, 512), ('wv', wv, 1024)):
                    r.op('pool', [lambda e, buf=buf, c0=c0, hp=hp: e.dma_start(
                        out=buf[0], in_=w_in[l][:, c0 + hp * 128:c0 + (hp + 1) * 128].rearrange("(k p) n -> p k n", p=128))],
                        writes=[f'{nm}'], dma=f'{nm}')

            def load_T(hp):
                bq = hp % 2
                for v in range(2):
                    for j in range(2):
                        q = (v * 2 + j) % 2
                        r.op('sp', [lambda e, hp=hp, v=v, j=j, q=q: e.dma_start(out=Tb32[q], in_=tb_d[l, hp, v, j])], writes=[f'Tb32{q}'], dma=f'tb{q}')
                        r.op('act', [lambda e, bq=bq, v=v, j=j, q=q: e.activation(out=T[bq][:, v, j, :], in_=Tb32[q], func=AF.Exp)], reads=[f'Tb32{q}'], writes=[f'T{bq}'])

            load(0)
            load_T(0)
            qblocks = range(4) if last else range(5)
            npairs = 16 if last else 18
            itc = 0
            def project(hp):
                b = hp % 2
                for bi in qblocks:
                    t0, sz = BLKS[bi]
                    pb = 6 + bi % 2
                    r.op('pe', [lambda e, k=k, pb=pb, t0=t0, sz=sz, b=b: e.matmul(bank(pb, sz), wq[b][:, k, :], aT[:, k, t0:t0 + sz], start=(k == 0), stop=(k == 7)) for k in range(8)],
                         reads=['wq'] + [f'aT:{k}:{bi}' for k in range(8)], writes=[PB(pb)])
                    ms = range(t0 // 128, (t0 + sz) // 128)
                    copy_any(qo[:, hp, t0:t0 + sz], bank(pb, sz), [], [PB(pb)] + [f'qo:{hp}:{m}:{j}' for m in ms for j in range(2)])
                for bi in range(5):
                    t0, sz = BLKS[bi]
                    pb = 6 + (bi + 1) % 2
                    r.op('pe', [lambda e, k=k, pb=pb, t0=t0, sz=sz, b=b: e.matmul(bank(pb, sz), wk[b][:, k, :], aT[:, k, t0:t0 + sz], start=(k == 0), stop=(k == 7)) for k in range(8)],
                         reads=['wk'] + [f'aT:{k}:{bi}' for k in range(8)], writes=[PB(pb)])
                    copy_any(kT[b][:, t0:t0 + sz], bank(pb, sz), [], [PB(pb), f'kT{b}'])
                for tile in range(18):
                    pb = 6 + tile % 2
                    bi = blk_of(tile * 128)
                    r.op('pe', [lambda e, k=k, pb=pb, tile=tile, b=b: e.matmul(bank(pb, 128), aT[:, k, tile * 128:(tile + 1) * 128], wv[b][:, k, :], start=(k == 0), stop=(k == 7)) for k in range(8)],
                         reads=['wv'] + [f'aT:{k}:{bi}' for k in range(8)], writes=[PB(pb)])
                    dst = mkap(V[b][:, tile, 0:64], [[128, 2], [1, 64]])
                    srcv = bank(pb, 128).rearrange("p (a n) -> p a n", a=2)
                    copy_any(dst, srcv, [], [PB(pb), f'V{b}'])
            for hp in range(4):
                b = hp % 2
                if hp == 0:
                    project(0)
                if hp + 1 < 4:
                    load(hp + 1)
                    load_T(hp + 1)
                iters = []
                for m in range(npairs):
                    if m < 16:
                        if 2 <= m <= 13:
                            tiles = [m + 2, m + 1, m, m - 1, m - 2]
                        elif m < 2:
                            tiles = [3, 2, 1, 0]
                        else:
                            tiles = [15, 14, 13, 12]
                        nw = len(tiles)
                        idx0 = 7 - (2 * tiles[0] - 2 * m)
                        tiles = tiles + [16, 17]
                    else:
                        tiles, nw, idx0 = [16, 17], 0, 0
                    for j in range(2):
                        iters.append((m, j, tiles, nw, idx0))

                def stage_a(itn, m, j, tiles, nw, idx0):
                    k2 = itn % 2
                    nt = len(tiles)
                    q0 = m * 128
                    pS = [0 + 2 * k2, 1 + 2 * k2]
                    Pb = P[k2]
                    rq = f'qo:{hp}:{m}:{j}'
                    r.op('pe', [lambda e, i=i, tl=tl, k2=k2, j=j, b=b, hp=hp, q0=q0: e.matmul(
                        ps[:, k2 * 1024 + i * 128:k2 * 1024 + (i + 1) * 128], kT[b][j * 64:(j + 1) * 64, tl * 128:(tl + 1) * 128],
                        qo[j * 64:(j + 1) * 64, hp, q0:q0 + 128], start=True, stop=True) for i, tl in enumerate(tiles)],
                        reads=[f'kT{b}', rq], writes=[PB(pS[0]), PB(pS[1])])
                    n0 = min(nt, 4) * 128
                    r.op('act', [lambda e, k2=k2, Pb=Pb, n0=n0: e.activation(out=Pb[:, 0:n0], in_=ps[:, k2 * 1024:k2 * 1024 + n0], func=AF.Exp, scale=0.125)],
                         writes=[PB(pS[0]), f'P{k2}'])
                    if nt > 4:
                        n1 = nt * 128
                        r.op('act', [lambda e, k2=k2, Pb=Pb, n1=n1: e.activation(out=Pb[:, 512:n1], in_=ps[:, k2 * 1024 + 512:k2 * 1024 + n1], func=AF.Exp, scale=0.125)],
                             writes=[PB(pS[1]), f'P{k2}'])
                    if nw > 0:
                        r.op('dve', [lambda e, Pb=Pb, nw=nw, idx0=idx0, j=j, b=b, m=m: e.tensor_tensor(
                            out=Pb[:, 0:nw * 128], in0=Pb[:, 0:nw * 128], in1=T[b][:, (1 if 2 <= m <= 13 else 0), j, idx0 * 64:idx0 * 64 + nw * 128], op=ALU.mult)],
                            reads=[f'T{b}'], writes=[f'P{k2}'])
                        if False:
                            r.op('pool', [lambda e, Pb=Pb: e.memset(Pb[0:64, 0:64], 0.0)], writes=[f'P{k2}'])
                            r.op('pool', [lambda e, Pb=Pb: e.memset(Pb[64:128, 0:128], 0.0)], writes=[f'P{k2}'])
                            r.op('pool', [lambda e, Pb=Pb: e.memset(Pb[0:64, 4 * 128 + 64:5 * 128], 0.0)], writes=[f'P{k2}'])

                def stage_b(itn, m, j, tiles, nw, idx0):
                    k2 = itn % 2
                    nt = len(tiles)
                    q0 = m * 128
                    pO = 4 + k2
                    Pb = P[k2]
                    rq = f'qo:{hp}:{m}:{j}'
                    c0, c1 = (0, 128) if j == 0 else (64, 192)
                    r.op('pe', [lambda e, i=i, tl=tl, Pb=Pb, pO=pO, b=b, nt=nt, c0=c0, c1=c1: e.matmul(
                        bank(pO, 128), V[b][:, tl, c0:c1], Pb[:, i * 128:(i + 1) * 128], start=(i == 0), stop=(i == nt - 1)) for i, tl in enumerate(tiles)],
                        reads=[f'V{b}', f'P{k2}'], writes=[PB(pO)])
                    rc = rec[k2]
                    if j == 0:
                        num, den, rcs = bank(pO, 128)[0:64, :], bank(pO, 128)[64:128, :], rc[0:64, :]
                    else:
                        num, den, rcs = bank(pO, 128)[64:128, :], bank(pO, 128)[0:64, :], rc[64:128, :]
                    r.op('dve', [lambda e, den=den, rcs=rcs: e.reciprocal(out=rcs, in_=den)], writes=[PB(pO), f'rec{k2}'])
                    r.op('dve', [lambda e, num=num, rcs=rcs, j=j, hp=hp, q0=q0: e.tensor_tensor(out=qo[j * 64:(j + 1) * 64, hp, q0:q0 + 128], in0=num, in1=rcs, op=ALU.mult)],
                         reads=[f'rec{k2}'], writes=[PB(pO), rq])

                stage_a(itc, *iters[0])
                for ii in range(len(iters)):
                    if ii + 1 < len(iters):
                        stage_a(itc + ii + 1, *iters[ii + 1])
                    stage_b(itc + ii, *iters[ii])
                    if ii == len(iters) // 3 and hp + 1 < 4:
                        project(hp + 1)
                itc += len(iters)

        def wout_phase(l, last):
            cvw = Carver()
            wo = cvw.bf16(8 * 1024).rearrange("p (k n) -> p k n", k=8)
            r.op('pool', [lambda e: e.dma_start(out=wo, in_=w_out[l].rearrange("(k p) n -> p k n", p=128))], writes=['wo'], dma='wo')
            it = 0
            for bi in (range(4) if last else range(5)):
                t0, sz = BLKS[bi]
                col = 1 if bi == 4 else 0
                ms = range(t0 // 128, (t0 + sz) // 128)
                rds = ['wo'] + [f'qo:{hp}:{m}:{j}' for hp in range(4) for m in ms for j in range(2)] + [f'gro:{c}:{bi}' for c in range(4)]
                for dc in range(8):
                    pb = it % 8
                    it += 1
                    r.op('pe', [lambda e, k=k, pb=pb, dc=dc, t0=t0, sz=sz: e.matmul(bank(pb, sz), wo[:, k, dc * 128:(dc + 1) * 128],
                                                                                    (qo[:, k, t0:t0 + sz] if k < 4 else gro[:, k - 4, t0:t0 + sz]),
                                                                                    start=(k == 0), stop=(k == 7)) for k in range(8)],
                         reads=rds, writes=[PB(pb)])
                    g1 = mod[:, l, 16 + dc, col:col + 1]
                    r.op('dve', [lambda e, pb=pb, dc=dc, t0=t0, sz=sz, g1=g1: e.scalar_tensor_tensor(
                        out=hT[:, dc, t0:t0 + sz], in0=bank(pb, sz), scalar=g1, in1=hT[:, dc, t0:t0 + sz],
                        op0=ALU.mult, op1=ALU.add)], reads=[f'mod{l}'], writes=[PB(pb), f'hT:{dc}:{bi}'])

        def ffn_phase(l, moe, blocks, cvf, gatesT=None):
            qof = qo[:].rearrange("p c t -> p (c t)")
            grf = gro[:].rearrange("p c t -> p (c t)")
            W1 = [qof[:, 0:4096].rearrange("p (k n) -> p k n", k=8), cvf.bf16(4096).rearrange("p (k n) -> p k n", k=8)]
            W3 = [qof[:, 4096:8192].rearrange("p (k n) -> p k n", k=8), cvf.bf16(4096).rearrange("p (k n) -> p k n", k=8)]
            W2 = [grf[:, 0:4096].rearrange("p (f n) -> p f n", f=4), cvf.bf16(4096).rearrange("p (f n) -> p f n", f=4)]
            G = [grf[:, 4096:6144].rearrange("p (f n) -> p f n", f=4), grf[:, 6144:8192].rearrange("p (f n) -> p f n", f=4)]
            S = [cvf.f32(512) for _ in range(2)]
            Tt = [cvf.f32(512) for _ in range(2)]
            gbc = [cvf.f32(NL) for _ in range(2)] if moe else None
            NE = 8 if moe else 1
            NG = DFF // 512
            seq = [(e_, g) for e_ in range(NE) for g in range(NG)]

            def load(i):
                e_, g = seq[i]
                b = i % 2
                w1s = (moe_w1[0, e_] if moe else ffn_w1[0])[:, g * 512:(g + 1) * 512].rearrange("(k p) n -> p k n", p=128)
                w3s = (moe_w3[0, e_] if moe else ffn_w3[0])[:, g * 512:(g + 1) * 512].rearrange("(k p) n -> p k n", p=128)
                w2s = (moe_w2[0, e_] if moe else ffn_w2[0])[g * 512:(g + 1) * 512, :].rearrange("(f p) n -> p f n", p=128)
                r.op('pool', [lambda e: e.dma_start(out=W1[b], in_=w1s)], writes=[f'W1{b}'], dma=f'W1{b}')
                r.op('pool', [lambda e: e.dma_start(out=W3[b], in_=w3s)], writes=[f'W3{b}'], dma=f'W3{b}')
                r.op('pool', [lambda e: e.dma_start(out=W2[b], in_=w2s)], writes=[f'W2{b}'], dma=f'W2{b}')

            load(0)
            ith = 0
            ito = 0
            itg = 0
            for i, (e_, g) in enumerate(seq):
                if i + 1 < len(seq):
                    load(i + 1)
                b = i % 2
                if moe and g == 0:
                    gb = gbc[e_ % 2]
                    sl = selb[e_ % 2]
                    r.op('sp', [lambda e, sl=sl, e_=e_: e.dma_start(out=sl[:], in_=sel_d[:, e_ * 128:(e_ + 1) * 128])], writes=[f'sel{e_ % 2}'], dma=f'sel{e_ % 2}')
                    for bi in blocks:
                        t0, sz = BLKS[bi]
                        pb = 4 + ito % 4
                        ito += 1
                        r.op('pe', [lambda e, pb=pb, t0=t0, sz=sz, sl=sl: e.matmul(bank(pb, sz), sl[0:8, :], gatesT[0:8, t0:t0 + sz], start=True, stop=True)],
                             reads=[f'sel{e_ % 2}', 'gatesT'], writes=[PB(pb)])
                        r.op('act', [lambda e, pb=pb, gb=gb, t0=t0, sz=sz: e.activation(out=gb[:, t0:t0 + sz], in_=bank(pb, sz), func=AF.Identity)],
                             writes=[PB(pb), f'gbc{e_ % 2}:{bi}'])
                for bi in blocks:
                    t0, sz = BLKS[bi]
                    col = 1 if bi == 4 else 0
                    Gb = G[itg % 2]
                    gname = f'G{itg % 2}'
                    itg += 1
                    for fc in range(4):
                        k2 = ith % 2
                        ith += 1
                        p1, p3 = k2, 2 + k2
                        r.op('pe', [lambda e, k=k, p1=p1, fc=fc, b=b, t0=t0, sz=sz: e.matmul(bank(p1, sz), W1[b][:, k, fc * 128:(fc + 1) * 128], aT[:, k, t0:t0 + sz],
                                                                                             start=(k == 0), stop=(k == 7)) for k in range(8)],
                             reads=[f'W1{b}'] + [f'aT:{k}:{bi}' for k in range(8)], writes=[PB(p1)])
                        r.op('pe', [lambda e, k=k, p3=p3, fc=fc, b=b, t0=t0, sz=sz: e.matmul(bank(p3, sz), W3[b][:, k, fc * 128:(fc + 1) * 128], aT[:, k, t0:t0 + sz],
                                                                                             start=(k == 0), stop=(k == 7)) for k in range(8)],
                             reads=[f'W3{b}'] + [f'aT:{k}:{bi}' for k in range(8)], writes=[PB(p3)])
                        s = S[k2]
                        r.op('act', [lambda e, p1=p1, s=s, sz=sz: e.activation(out=s[:, :sz], in_=bank(p1, sz), func=AF.Silu)], writes=[PB(p1), f'S{k2}'])
                        if moe:
                            tt = Tt[k2]
                            gbs = gbc[e_ % 2]
                            r.op('dve', [lambda e, p3=p3, tt=tt, gbs=gbs, t0=t0, sz=sz: e.tensor_tensor(out=tt[:, :sz], in0=bank(p3, sz), in1=gbs[:, t0:t0 + sz], op=ALU.mult)],
                                 reads=[f'gbc{e_ % 2}:{bi}'], writes=[PB(p3), f'Tt{k2}'])
                            r.op('dve', [lambda e, tt=tt, s=s, fc=fc, Gb=Gb, sz=sz: e.tensor_tensor(out=Gb[:, fc, :sz], in0=tt[:, :sz], in1=s[:, :sz], op=ALU.mult)],
                                 reads=[f'Tt{k2}', f'S{k2}'], writes=[gname])
                        else:
                            r.op('dve', [lambda e, p3=p3, s=s, fc=fc, Gb=Gb, sz=sz: e.tensor_tensor(out=Gb[:, fc, :sz], in0=bank(p3, sz), in1=s[:, :sz], op=ALU.mult)],
                                 reads=[f'S{k2}'], writes=[PB(p3), gname])
                    for dc in range(8):
                        pb = 4 + ito % 4
                        ito += 1
                        r.op('pe', [lambda e, fc=fc, pb=pb, dc=dc, Gb=Gb, b=b, sz=sz: e.matmul(bank(pb, sz), W2[b][:, fc, dc * 128:(dc + 1) * 128], Gb[:, fc, :sz],
                                                                                               start=(fc == 0), stop=(fc == 3)) for fc in range(4)],
                             reads=[f'W2{b}', gname], writes=[PB(pb)])
                        g2 = mod[:, l, 40 + dc, col:col + 1]
                        r.op('dve', [lambda e, pb=pb, dc=dc, t0=t0, sz=sz, g2=g2: e.scalar_tensor_tensor(
                            out=hT[:, dc, t0:t0 + sz], in0=bank(pb, sz), scalar=g2, in1=hT[:, dc, t0:t0 + sz],
                            op0=ALU.mult, op1=ALU.add)], reads=[f'mod{l}'], writes=[PB(pb), f'hT:{dc}:{bi}'])

        def moe_gates(cvm, logitsT, gatesT):
            lg = cvm.f32(128).rearrange("p (t e) -> p t e", e=8)
            l2 = cvm.f32(128).rearrange("p (t e) -> p t e", e=8)
            eq1 = cvm.f32(128).rearrange("p (t e) -> p t e", e=8)
            eq2 = cvm.f32(128).rearrange("p (t e) -> p t e", e=8)
            gt = cvm.f32(128).rearrange("p (t e) -> p t e", e=8)
            m1 = cvm.f32(16)
            m2 = cvm.f32(16)
            w1 = cvm.f32(16)
            w2 = cvm.f32(16)

            def bc(v):
                return mkap(v[:, 0:1], [[1, 16], [0, 8]])

            r.op('pe', [lambda e, t=t: e.transpose(out=bank(0, 8, t * 8), in_=logitsT[0:8, t * 128:(t + 1) * 128], identity=ident[0:8, 0:8]) for t in range(16)],
                 reads=['logitsT', 'ident'], writes=[PB(0)])
            r.op('dve', [lambda e: e.tensor_copy(out=lg, in_=bank(0, 128).rearrange("p (t e) -> p t e", e=8))], writes=[PB(0), 'lg'])
            AX = mybir.AxisListType.X
            r.op('dve', [lambda e: e.tensor_reduce(out=m1, in_=lg, axis=AX, op=ALU.max)], writes=['lg', 'm1'])
            r.op('dve', [lambda e: e.tensor_tensor(out=eq1, in0=lg, in1=bc(m1), op=ALU.is_equal)], writes=['lg', 'm1', 'eq1'])
            r.op('dve', [lambda e: e.scalar_tensor_tensor(out=l2, in0=eq1, scalar=cst[:, 2:3], in1=lg, op0=ALU.mult, op1=ALU.add)], reads=['cst'], writes=['eq1', 'lg', 'l2'])
            r.op('dve', [lambda e: e.tensor_reduce(out=m2, in_=l2, axis=AX, op=ALU.max)], writes=['l2', 'm2'])
            r.op('dve', [lambda e: e.tensor_tensor(out=eq2, in0=l2, in1=bc(m2), op=ALU.is_equal)], writes=['l2', 'm2', 'eq2'])
            r.op('dve', [lambda e: e.tensor_tensor(out=w2, in0=m2, in1=m1, op=ALU.subtract)], writes=['m1', 'm2', 'w2'])
            r.op('act', [lambda e: e.activation(out=w2, in_=w2, func=AF.Exp)], writes=['w2'])
            r.op('act', [lambda e: e.activation(out=w1, in_=w2, func=AF.Identity, bias=cst[:, 1:2])], reads=['cst'], writes=['w2', 'w1'])
            r.op('dve', [lambda e: e.reciprocal(out=w1, in_=w1)], writes=['w1'])
            r.op('dve', [lambda e: e.tensor_tensor(out=w2, in0=w2, in1=w1, op=ALU.mult)], writes=['w1', 'w2'])
            r.op('dve', [lambda e: e.tensor_tensor(out=eq1, in0=eq1, in1=bc(w1), op=ALU.mult)], writes=['eq1', 'w1'])
            r.op('dve', [lambda e: e.tensor_tensor(out=eq2, in0=eq2, in1=bc(w2), op=ALU.mult)], writes=['eq2', 'w2'])
            r.op('dve', [lambda e: e.tensor_tensor(out=gt, in0=eq1, in1=eq2, op=ALU.add)], writes=['eq1', 'eq2', 'gt'])
            for q in range(4):
                r.op('pe', [lambda e, t=t, q=q: e.transpose(out=bank(q, 128, (t % 4) * 128)[0:8, :], in_=gt[:, t, :], identity=ident[:]) for t in range(q * 4, q * 4 + 4)],
                     reads=['gt', 'ident'], writes=[PB(q)])
                r.op('dve', [lambda e, q=q: e.tensor_copy(out=gatesT[0:8, q * 512:(q + 1) * 512], in_=bank(q)[0:8, :])], writes=[PB(q), 'gatesT'])

        def moe_gates_sparse(cvm, logitsT, posmb, gtp, flags_i, iota):
            def t3(n=128):
                return cvm.f32(n).rearrange("p (t e) -> p t e", e=8)
            lg, l2, eq1, eq2, msk, pref, tot, off, pos = (t3() for _ in range(9))
            m1, m2, w1, w2 = (cvm.f32(16) for _ in range(4))
            ne = cvm.f32(8)
            flagsf = cvm.f32(32).rearrange("p (e b) -> p e b", b=4)
            ustr = cvm.f32(128)
            ones1 = cvm.f32(128)
            AX = mybir.AxisListType.X

            def bc(v):
                return mkap(v[:, 0:1], [[1, 16], [0, 8]])

            def fl(v):
                return v.rearrange("p t e -> p (t e)")

            r.op('sp', [lambda e: e.dma_start(out=ustr, in_=ustr_d)], writes=['ustr'], dma='c5')
            r.op('sp', [lambda e: e.dma_start(out=iota, in_=iota_d)], writes=['iota'], dma='c6')
            r.op('pool', [lambda e: e.memset(ones1, 1.0)], writes=['ones1'])
            r.op('pe', [lambda e, t=t: e.transpose(out=bank(0, 8, t * 8), in_=logitsT[0:8, t * 128:(t + 1) * 128], identity=ident[0:8, 0:8]) for t in range(16)],
                 reads=['logitsT', 'ident'], writes=[PB(0)])
            D = 'gsp'
            r.op('dve', [lambda e: e.tensor_copy(out=lg, in_=bank(0, 128).rearrange("p (t e) -> p t e", e=8))], writes=[PB(0), D])
            r.op('dve', [lambda e: e.tensor_reduce(out=m1, in_=lg, axis=AX, op=ALU.max)], writes=[D])
            r.op('dve', [lambda e: e.tensor_tensor(out=eq1, in0=lg, in1=bc(m1), op=ALU.is_equal)], writes=[D])
            r.op('dve', [lambda e: e.scalar_tensor_tensor(out=l2, in0=eq1, scalar=cst[:, 2:3], in1=lg, op0=ALU.mult, op1=ALU.add)], reads=['cst'], writes=[D])
            r.op('dve', [lambda e: e.tensor_reduce(out=m2, in_=l2, axis=AX, op=ALU.max)], writes=[D])
            r.op('dve', [lambda e: e.tensor_tensor(out=eq2, in0=l2, in1=bc(m2), op=ALU.is_equal)], writes=[D])
            r.op('dve', [lambda e: e.tensor_tensor(out=msk, in0=eq1, in1=eq2, op=ALU.add)], writes=[D])
            r.op('dve', [lambda e: e.tensor_tensor(out=w2, in0=m2, in1=m1, op=ALU.subtract)], writes=[D])
            r.op('act', [lambda e: e.activation(out=w2, in_=w2, func=AF.Exp)], writes=[D])
            r.op('act', [lambda e: e.activation(out=w1, in_=w2, func=AF.Identity, bias=cst[:, 1:2])], reads=['cst'], writes=[D])
            r.op('dve', [lambda e: e.reciprocal(out=w1, in_=w1)], writes=[D])
            r.op('dve', [lambda e: e.tensor_tensor(out=w2, in0=w2, in1=w1, op=ALU.mult)], writes=[D])
            r.op('dve', [lambda e: e.tensor_tensor(out=eq1, in0=eq1, in1=bc(w1), op=ALU.mult)], writes=[D])
            r.op('dve', [lambda e: e.tensor_tensor(out=eq2, in0=eq2, in1=bc(w2), op=ALU.mult)], writes=[D])
            r.op('dve', [lambda e: e.tensor_tensor(out=gtp, in0=eq1, in1=eq2, op=ALU.add)], writes=[D, 'gtp'])
            r.op('pe', [lambda e: e.matmul(bank(1, 128), ustr, fl(msk), start=True, stop=True)], reads=[D, 'ustr'], writes=[PB(1)])
            r.op('pe', [lambda e: e.matmul(bank(2, 128), ones1, fl(msk), start=True, stop=True)], reads=[D, 'ones1'], writes=[PB(2)])
            r.op('dve', [lambda e: e.tensor_copy(out=fl(pref), in_=bank(1, 128))], writes=[PB(1), D])
            r.op('dve', [lambda e: e.tensor_copy(out=fl(tot), in_=bank(2, 128))], writes=[PB(2), D])
            r.op('dve', [lambda e: e.memset(off[:, 0, :], 0.0)], writes=[D])
            for t in range(1, 16):
                r.op('dve', [lambda e, t=t: e.tensor_tensor(out=off[:, t, :], in0=off[:, t - 1, :], in1=tot[:, t - 1, :], op=ALU.add)], writes=[D])
            r.op('dve', [lambda e: e.tensor_tensor(out=ne, in0=off[:, 15, :], in1=tot[:, 15, :], op=ALU.add)], writes=[D])
            r.op('dve', [lambda e: e.tensor_tensor(out=pos, in0=pref, in1=off, op=ALU.add)], writes=[D])
            r.op('dve', [lambda e: e.scalar_tensor_tensor(out=pos, in0=pos, scalar=cst[:, 1:2], in1=msk, op0=ALU.add, op1=ALU.mult)], reads=['cst'], writes=[D])
            for b_ in range(4):
                r.op('dve', [lambda e, b_=b_: e.tensor_scalar(out=posmb[:, b_, :, :], in0=pos, scalar1=cst[:, 4:5], scalar2=cst[:, 9 + b_:10 + b_], op0=ALU.add, op1=ALU.add)],
                     reads=['cst'], writes=[D, 'posmb'])
                r.op('dve', [lambda e, b_=b_: e.tensor_scalar(out=flagsf[:, :, b_], in0=ne, scalar1=cst[:, 5 + b_:6 + b_], scalar2=cst[:, 1:2], op0=ALU.is_gt, op1=ALU.mult)],
                     reads=['cst'], writes=[D])
            r.op('dve', [lambda e: e.tensor_copy(out=flags_i[:], in_=flagsf.rearrange("p e b -> p (e b)"))], writes=[D, 'flags'])

        def moe_sparse_phase(l, cvm, posmb, gtp, flags_i, iota):
            aTf = aT[:].rearrange("p c t -> p (c t)")
            W1 = [aTf[:, i * 6144:i * 6144 + 2048].rearrange("p (k n) -> p k n", k=8) for i in range(2)]
            W3 = [aTf[:, i * 6144 + 2048:i * 6144 + 4096].rearrange("p (k n) -> p k n", k=8) for i in range(2)]
            W2 = [aTf[:, i * 6144 + 4096:i * 6144 + 6144].rearrange("p (f n) -> p f n", f=2) for i in range(2)]
            G = [aTf[:, 12288 + i * 1024:12288 + (i + 1) * 1024].rearrange("p (f n) -> p f n", f=2) for i in range(2)]
            a2g = aTf[:, 14336:18432].rearrange("p (c n) -> p c n", c=8)
            Selb = [cvm.bf16(512) for _ in range(4)]
            SelT = [cvm.bf16(2048) for _ in range(2)]
            outS = cvm.f32(4096).rearrange("p (s n) -> p s n", s=4)
            outSb = cvm.bf16(4096).rearrange("p (s n) -> p s n", s=4)
            S = [cvm.f32(512) for _ in range(2)]
            psTb = ps[:, 0:1024].bitcast(BF16)
            cn = {'sel': 0, 'w': 0, 'h': 0, 'g': 0, 'st': 0}
            NGRP = DFF // 256
            for e_ in range(8):
                for b_ in range(4):
                    kf = e_ * 4 + b_
                    if b_ in (1, 2):
                        r.begin_region(flags_i[0:1, kf:kf + 1], 'flags')
                    for t in range(16):
                        si = cn['sel'] % 4
                        cn['sel'] += 1
                        sbuf_ = Selb[si]
                        r.op('dve', [lambda e, sbuf_=sbuf_, t=t, b_=b_, e_=e_: e.tensor_scalar(
                            out=sbuf_, in0=iota, scalar1=posmb[:, b_, t, e_:e_ + 1], scalar2=cst[:, 1:2], op0=ALU.is_equal, op1=ALU.mult)],
                            reads=['posmb', 'iota', 'cst'], writes=[f'Sel{si}'])
                        r.op('pe', [lambda e, c=c, t=t, sbuf_=sbuf_: e.matmul(bank(c), a2tok(t)[:, c * 128:(c + 1) * 128], sbuf_, start=(t == 0), stop=(t == 15)) for c in range(8)],
                             reads=[f'Sel{si}', f'a2tok{t}'], writes=[PB(c) for c in range(8)])
                    for c in range(8):
                        copy_any(a2g[:, c, :], bank(c), [], [PB(c), f'a2g{c}'])
                    for g in range(NGRP):
                        wb = cn['w'] % 2
                        cn['w'] += 1
                        w1s = moe_w1[0, e_][:, g * 256:(g + 1) * 256].rearrange("(k p) n -> p k n", p=128)
                        w3s = moe_w3[0, e_][:, g * 256:(g + 1) * 256].rearrange("(k p) n -> p k n", p=128)
                        w2s = moe_w2[0, e_][g * 256:(g + 1) * 256, :].rearrange("(f p) n -> p f n", p=128)
                        r.op('pool', [lambda e, wb=wb, w1s=w1s: e.dma_start(out=W1[wb], in_=w1s)], writes=[f'W1{wb}'], dma=f'W1{wb}')
                        r.op('pool', [lambda e, wb=wb, w3s=w3s: e.dma_start(out=W3[wb], in_=w3s)], writes=[f'W3{wb}'], dma=f'W3{wb}')
                        r.op('pool', [lambda e, wb=wb, w2s=w2s: e.dma_start(out=W2[wb], in_=w2s)], writes=[f'W2{wb}'], dma=f'W2{wb}')
                        gi = cn['g'] % 2
                        cn['g'] += 1
                        for fc in range(2):
                            k2 = cn['h'] % 2
                            cn['h'] += 1
                            p1, p3 = k2, 2 + k2
                            r.op('pe', [lambda e, kk=kk, p1=p1, fc=fc, wb=wb: e.matmul(bank(p1), W1[wb][:, kk, fc * 128:(fc + 1) * 128], a2g[:, kk, :],
                                                                                      start=(kk == 0), stop=(kk == 7)) for kk in range(8)],
                                 reads=[f'W1{wb}'] + [f'a2g{kk}' for kk in range(8)], writes=[PB(p1)])
                            r.op('pe', [lambda e, kk=kk, p3=p3, fc=fc, wb=wb: e.matmul(bank(p3), W3[wb][:, kk, fc * 128:(fc + 1) * 128], a2g[:, kk, :],
                                                                                      start=(kk == 0), stop=(kk == 7)) for kk in range(8)],
                                 reads=[f'W3{wb}'] + [f'a2g{kk}' for kk in range(8)], writes=[PB(p3)])
                            sk = S[k2]
                            r.op('act', [lambda e, p1=p1, sk=sk: e.activation(out=sk, in_=bank(p1), func=AF.Silu)], writes=[PB(p1), f'S{k2}'])
                            r.op('dve', [lambda e, p3=p3, sk=sk, fc=fc, gi=gi: e.tensor_tensor(out=G[gi][:, fc, :], in0=bank(p3), in1=sk, op=ALU.mult)],
                                 reads=[f'S{k2}'], writes=[PB(p3), f'G{gi}'])
                        for s_ in range(4):
                            for dh in range(2):
                                pb = 4 + (s_ % 2) * 2 + dh
                                r.op('pe', [lambda e, fc=fc, pb=pb, s_=s_, dh=dh, gi=gi, wb=wb: e.matmul(
                                    bank(pb), G[gi][:, fc, s_ * 128:(s_ + 1) * 128], W2[wb][:, fc, dh * 512:(dh + 1) * 512], start=(fc == 0), stop=(fc == 1)) for fc in range(2)],
                                    reads=[f'G{gi}', f'W2{wb}'], writes=[PB(pb)])
                                dst = outS[:, s_, dh * 512:(dh + 1) * 512]
                                if g == 0:
                                    r.op('dve', [lambda e, pb=pb, dst=dst: e.tensor_copy(out=dst, in_=bank(pb))], writes=[PB(pb), f'outS{s_}'])
                                else:
                                    r.op('dve', [lambda e, pb=pb, dst=dst: e.tensor_tensor(out=dst, in0=bank(pb), in1=dst, op=ALU.add)], writes=[PB(pb), f'outS{s_}'])
                    for s_ in range(4):
                        copy_any(outSb[:, s_, :], outS[:, s_, :], [f'outS{s_}'], [f'outSb{s_}'])
                    for tb in range(4):
                        kt = cn['st'] % 2
                        cn['st'] += 1
                        for tt in range(4):
                            t = tb * 4 + tt
                            si = cn['sel'] % 4
                            cn['sel'] += 1
                            sbuf_ = Selb[si]
                            r.op('dve', [lambda e, sbuf_=sbuf_, t=t, b_=b_, e_=e_: e.tensor_scalar(
                                out=sbuf_, in0=iota, scalar1=posmb[:, b_, t, e_:e_ + 1], scalar2=gtp[:, t, e_:e_ + 1], op0=ALU.is_equal, op1=ALU.mult)],
                                reads=['posmb', 'iota', 'gtp'], writes=[f'Sel{si}'])
                            r.op('pe', [lambda e, s_=s_, tt=tt, sbuf_=sbuf_: e.transpose(out=psTb[:, s_ * 512 + tt * 128:s_ * 512 + (tt + 1) * 128],
                                                                                         in_=sbuf_[:, s_ * 128:(s_ + 1) * 128], identity=identb[:]) for s_ in range(4)],
                                 reads=[f'Sel{si}', 'identb'], writes=[PB(0), PB(1)])
                        copy_any(SelT[kt], psTb, [], [PB(0), PB(1), f'SelT{kt}'])
                        stv = SelT[kt].rearrange("p (s n) -> p s n", s=4)
                        for c in range(8):
                            pb = 4 + c % 4
                            r.op('pe', [lambda e, s_=s_, c=c, pb=pb, stv=stv: e.matmul(bank(pb), outSb[:, s_, c * 128:(c + 1) * 128], stv[:, s_, :],
                                                                                      start=(s_ == 0), stop=(s_ == 3)) for s_ in range(4)],
                                 reads=[f'outSb{s_}' for s_ in range(4)] + [f'SelT{kt}'], writes=[PB(pb)])
                            g2 = mod[:, l, 40 + c, 0:1]
                            r.op('dve', [lambda e, pb=pb, c=c, tb=tb, g2=g2: e.scalar_tensor_tensor(
                                out=hT[:, c, tb * 512:(tb + 1) * 512], in0=bank(pb), scalar=g2, in1=hT[:, c, tb * 512:(tb + 1) * 512],
                                op0=ALU.mult, op1=ALU.add)], reads=[f'mod{l}'], writes=[PB(pb), f'hT:{c}:{tb}'])
                    if b_ in (1, 3):
                        r.end_region()

        def final_phase():
            cvf = Carver()
            sq = [cvf.f32(512) for _ in range(2)]
            lnv = cvf.f32(512)
            rs = cvf.f32(512)
            tmA = cvf.f32(8 * 512).rearrange("p (c n) -> p c n", c=8)
            ost = [cvf.f32(1024) for _ in range(2)]
            for bi in range(4):
                t0, sz = BLKS[bi]
                for c in range(8):
                    s = sq[c % 2]
                    r.op('act', [lambda e, s=s, c=c, t0=t0, sz=sz: e.activation(out=s, in_=hT[:, c, t0:t0 + sz], func=AF.Square)], reads=[f'hT:{c}:{bi}'], writes=[f'sq{c % 2}'])
                    r.op('pe', [lambda e, s=s, c=c: e.matmul(bank(0), onesf[:], s, start=(c == 0), stop=(c == 7))], reads=[f'sq{c % 2}', 'onesf'], writes=[PB(0)])
                r.op('act', [lambda e: e.activation(out=lnv, in_=bank(0), func=AF.Ln, bias=cst[:, 0:1])], reads=['cst'], writes=[PB(0), 'lnv'])
                r.op('act', [lambda e: e.activation(out=rs, in_=lnv, func=AF.Exp, scale=-0.5)], reads=['lnv'], writes=['rs'])
                for c in range(8):
                    r.op('dve', [lambda e, c=c, t0=t0, sz=sz: e.scalar_tensor_tensor(out=tmA[:, c, :], in0=hT[:, c, t0:t0 + sz], scalar=vcol(R_FINALG + c), in1=rs,
                                                                                     op0=ALU.mult, op1=ALU.mult)], reads=[f'hT:{c}:{bi}', 'rs', 'vT'], writes=[f'tmA{c}'])
                for tt in range(4):
                    tile = bi * 4 + tt
                    k2 = tile % 2
                    for half in range(2):
                        pb = 2 + k2 * 2 + half
                        r.op('pe', [lambda e, c=c, pb=pb, tt=tt: e.transpose(out=bank(pb, 128, (c % 4) * 128), in_=tmA[:, c, tt * 128:(tt + 1) * 128], identity=ident[:])
                                    for c in range(half * 4, half * 4 + 4)], reads=[f'tmA{c}' for c in range(half * 4, half * 4 + 4)] + ['ident'], writes=[PB(pb)])
                        copy_any(ost[k2][:, half * 512:(half + 1) * 512], bank(pb), [], [PB(pb), f'ost{k2}'])
                    r.op('sp', [lambda e, k2=k2, tile=tile: e.dma_start(out=out_d[tile * 128:(tile + 1) * 128, :], in_=ost[k2])],
                         reads=[f'ost{k2}'], writes=[f'out{tile}'], dma=f'out{k2}')
            r.op('sp', [], reads=[f'out{t}' for t in range(16)], final=True)

        adaln(0)
        adaln(1)
        r.barrier()
        done = False
        if debug == 'load':
            dump_hT_and_finish()
            done = True
        for l in range(2):
            if done:
                break
            last = (l == 1)
            norm_mod(l, 0, range(5), Carver())
            if debug == f'a1_{l}':
                dump_bf16(aT, 8, lambda c: []); done = True; break
            r.barrier()
            lru_phase(l, last)
            if debug == f'lru_{l}':
                dump_bf16(gro, 4, lambda c: []); done = True; break
            r.barrier()
            attn_phase(l, last)
            if debug == f'attn_{l}':
                dump_bf16(qo, 4, lambda c: []); done = True; break
            r.barrier()
            wout_phase(l, last)
            if debug == f'wout_{l}':
                dump_hT_and_finish(); done = True; break
            r.barrier()
            if not last:
                norm_mod(l, 1, range(5), Carver())
                r.barrier()
                ffn_phase(l, False, range(5), Carver())
            else:
                cvm = Carver()
                if MOE_SPARSE:
                    posmb = cvm.f32(512).rearrange("p (b t e) -> p b t e", b=4, t=16)
                    gtp = cvm.f32(128).rearrange("p (t e) -> p t e", e=8)
                    iota = cvm.f32(512)
                    keep = cvm.off
                    logitsT = cvm.f32(NL)
                    norm_mod(l, 1, range(4), cvm, router=True, logitsT=logitsT)
                    moe_gates_sparse(cvm, logitsT, posmb, gtp, flags_i, iota)
                    r.barrier()
                    cvm.off = keep
                    moe_sparse_phase(l, cvm, posmb, gtp, flags_i, iota)
                else:
                    gatesT = cvm.f32(NL)
                    logitsT = cvm.f32(NL)
                    norm_mod(l, 1, range(4), cvm, router=True, logitsT=logitsT)
                    moe_gates(cvm, logitsT, gatesT)
                    r.barrier()
                    cvm.off = NL
                    ffn_phase(l, True, range(4), cvm, gatesT=gatesT)
            if debug == f'ffn_{l}':
                dump_hT_and_finish(); done = True; break
            r.barrier()
        if not done:
            final_phase()
        emit(nc, r, es)
    return nc


def _host_tables(inp):
    f32 = np.float32
    kc = np.arange(64)[:, None]
    qc = np.arange(64)[None, :]
    dcm = np.clip(kc - qc, -15, 15) + 15
    cs = np.clip(qc - 8, 0, 48)
    ok = (kc >= cs) & (kc < cs + 16)
    tb = np.full((2, 4, 2, 2, 128, 16, 64), NEG, f32)
    rpb = np.asarray(inp['na_rpb'], f32)
    for l in range(2):
        for h in range(8):
            hp, j = divmod(h, 2)
            for half in range(2):
                for idx in range(16):
                    d = (7 - idx) if half == 0 else (8 - idx)
                    if -7 <= d <= 7:
                        vals = rpb[l, h, d + 7][dcm]
                        tb[l, hp, 0, j, half * 64:(half + 1) * 64, idx, :] = np.where(ok, vals, f32(NEG))
                        if -4 <= d <= 3:
                            tb[l, hp, 1, j, half * 64:(half + 1) * 64, idx, :] = np.where(ok, vals, f32(NEG))
    tb = tb.reshape(2, 4, 2, 2, 128, 1024)
    wbd = np.zeros((2, 2, 2, 4, 128, 128), f32)
    for a, nm in enumerate(('lru_wa', 'lru_wx')):
        w = np.asarray(inp[nm], f32)
        for c in range(4):
            for s in range(2):
                wbd[:, :, a, c, s * 64:(s + 1) * 64, s * 64:(s + 1) * 64] = w[:, :, c * 2 + s]
    ident = np.eye(128, dtype=f32)
    sel = np.zeros((8, 8, 128), f32)
    for e in range(8):
        sel[e, e, :] = 1.0
    sel = sel.reshape(8, 1024)
    return tb, wbd, ident, sel


def _consts():
    f32 = np.float32
    k = np.arange(128)
    ustrict = (k[:, None] < k[None, :]).astype(f32)
    iota = np.ascontiguousarray(np.broadcast_to(np.arange(512, dtype=f32)[None, :], (128, 512)))
    return ustrict, iota


def _vecs(inp, b):
    f32 = np.float32
    rows = []
    for l in range(2):
        rows.append(np.asarray(inp['ada_b'][l], f32).reshape(48, 128))
        rows.append(np.asarray(inp['mix_norm_g'][l], f32).reshape(8, 128))
        rows.append(np.asarray(inp['ffn_norm_g'][l], f32).reshape(8, 128))
        rows.append(np.asarray(inp['conv_w'][l], f32).reshape(16, 128))
        rows.append(np.asarray(inp['conv_b'][l], f32).reshape(4, 128))
        rows.append(np.asarray(inp['lru_ba'][l], f32).reshape(8, 128))
        rows.append(np.asarray(inp['lru_bx'][l], f32).reshape(8, 128))
        rows.append(np.asarray(inp['lru_lam'][l], f32).reshape(8, 128))
    rows.append(np.asarray(inp['final_g'], f32).reshape(8, 128))
    rows.append(np.asarray(inp['c'][b], f32).reshape(8, 128))
    rows.append(np.asarray(inp['c_ctx'], f32).reshape(8, 128))
    v = np.concatenate(rows, 0)
    out = np.zeros((256, 128), f32)
    out[:v.shape[0]] = v
    return out


_NC_CACHE = {}


def make_in_maps(inp, cores):
    tb, wbd, ident, sel = _host_tables(inp)
    f32 = np.float32
    shared = {
        'ada_w': np.ascontiguousarray(inp['ada_w'], f32), 'w_in': np.ascontiguousarray(inp['w_in'], f32),
        'w_out': np.ascontiguousarray(inp['w_out'], f32),
        'ffn_w1': np.ascontiguousarray(inp['ffn_w1'], f32), 'ffn_w3': np.ascontiguousarray(inp['ffn_w3'], f32),
        'ffn_w2': np.ascontiguousarray(inp['ffn_w2'], f32),
        'moe_w1': np.ascontiguousarray(inp['moe_w1'], f32), 'moe_w3': np.ascontiguousarray(inp['moe_w3'], f32),
        'moe_w2': np.ascontiguousarray(inp['moe_w2'], f32),
        'moe_router': np.ascontiguousarray(inp['moe_router'], f32),
        'rb': np.ascontiguousarray(np.asarray(inp['moe_router_b'], f32).reshape(8, 1)),
        'wbd': wbd, 'tb': tb, 'ident': ident, 'sel': sel,
        'ustrict': _consts()[0], 'iota': _consts()[1],
    }
    maps = []
    for b in cores:
        m = dict(shared)
        m['x'] = np.ascontiguousarray(inp['x'][b], f32)
        m['ctx'] = np.ascontiguousarray(inp['ctx'][b], f32)
        m['vecs'] = _vecs(inp, b)
        maps.append(m)
    return maps


def kernel(**inputs):
    inp = {k: np.asarray(v) for k, v in inputs.items()}
    if 'nc' not in _NC_CACHE:
        _NC_CACHE['nc'] = build_program()
    nc = _NC_CACHE['nc']
    maps = make_in_maps(inp, range(8))
    res = run_bass_kernel_spmd(nc, maps, core_ids=list(range(8)))
    out = np.stack([np.asarray(res.results[b]['out'], np.float32) for b in range(8)], 0)
    return out
```

```python
import numpy as np
from contextlib import ExitStack
import concourse.bass as bass
import concourse.mybir as mybir
from concourse.bass_utils import run_bass_kernel_spmd

F32 = mybir.dt.float32
BF16 = mybir.dt.bfloat16
AF = mybir.ActivationFunctionType
ALU = mybir.AluOpType

ENGINES = ('pe', 'act', 'dve', 'pool', 'sp')


class Rec:
    def __init__(self):
        self.ops = {e: [] for e in ENGINES}
        self.cnt = {}
        self.last_w = {}
        self.readers = {}
        self.waited = {e: {} for e in ENGINES}
        self.region = None
        self.pre_counts = {}
        self.regions = []
        self._snap = None

    def begin_region(self, flag_ap, flag_res):
        for eng in ENGINES:
            self.op(eng, [], reads=[flag_res], final=True)
        self.regions.append(flag_ap)
        self.region = len(self.regions) - 1
        self._snap = {e: dict(w) for e, w in self.waited.items()}

    def end_region(self):
        self.region = None
        self.waited = self._snap
        self._snap = None

    def op(self, eng, fns, reads=(), writes=(), dma=None, final=False):
        deps = {}

        def need(cv):
            c, v = cv
            if deps.get(c, 0) < v:
                deps[c] = v

        for r in reads:
            if r in self.last_w:
                need(self.last_w[r])
        for w in writes:
            if w in self.last_w:
                need(self.last_w[w])
            for cv in self.readers.get(w, {}).items():
                need(cv)
        waits = []
        for c, v in deps.items():
            if c == 'pe' and eng == 'pe' and dma is None:
                continue
            if self.waited[eng].get(c, 0) >= v:
                continue
            self.waited[eng][c] = v
            waits.append((c, v))
        if final:
            self.ops[eng].append((waits, [], None, 0, self.region))
            return
        ctr = ('dma:' + dma) if dma else eng
        step = 16 if dma else 1
        if self.region is not None:
            d_ = self.pre_counts.setdefault((eng, self.region), {})
            if ctr not in d_:
                d_[ctr] = self.cnt.get(ctr, 0)
        val = self.cnt.get(ctr, 0) + step
        self.cnt[ctr] = val
        self.ops[eng].append((waits, list(fns), ctr, step, self.region))
        for r in reads:
            self.readers.setdefault(r, {})[ctr] = val
        for w in writes:
            self.last_w[w] = (ctr, val)
            self.readers[w] = {}

    def barrier(self):
        for eng in ENGINES:
            waits = []
            for c, v in self.cnt.items():
                if self.waited[eng].get(c, 0) >= v:
                    continue
                self.waited[eng][c] = v
                waits.append((c, v))
            self.ops[eng].append((waits, [], None, 0, None))
        self.last_w = {}
        self.readers = {}


def emit(nc, rec, es):
    names = sorted(rec.cnt.keys())
    sems = {n: es.enter_context(nc.semaphore(n.replace(':', '_'))) for n in names}
    block = es.enter_context(nc.Block())

    def run_ops(e, ops):
        for waits, fns, ctr, step, _ in ops:
            for c, v in waits:
                e.wait_ge(sems[c], v)
            ins = None
            for fn in fns:
                ins = fn(e)
            if ins is not None and ctr is not None:
                ins.then_inc(sems[ctr], step)

    def replay(e, eng):
        ops = rec.ops[eng]
        if not rec.regions:
            run_ops(e, ops)
            return
        with e.register(f"flag_{eng}") as reg:
            i = 0
            while i < len(ops):
                rg = ops[i][4]
                j = i
                while j < len(ops) and ops[j][4] == rg:
                    j += 1
                run = ops[i:j]
                i = j
                if rg is None:
                    run_ops(e, run)
                    continue
                tot, pre = {}, {}
                cur = {}
                for waits, fns, ctr, step, _ in run:
                    if ctr is None:
                        continue
                    tot[ctr] = tot.get(ctr, 0) + step
                if not tot:
                    continue
                for c in tot:
                    pre[c] = rec.pre_counts[(eng, rg)][c]
                e.reg_load(reg, rec.regions[rg])
                with e.If_ne(reg, 0):
                    run_ops(e, run)
                with e.Else():
                    for c in tot:
                        e.wait_ge(sems[c], pre[c])
                        e.sem_inc(sems[c], tot[c])

    @block.tensor
    def _(e):
        replay(e, 'pe')

    @block.scalar
    def _(e):
        replay(e, 'act')

    @block.vector
    def _(e):
        replay(e, 'dve')

    @block.gpsimd
    def _(e):
        replay(e, 'pool')

    @block.sync
    def _(e):
        replay(e, 'sp')


NT = 2304
NL = 2048
BLKS = [(0, 512), (512, 512), (1024, 512), (1536, 512), (2048, 256)]
EPS = 1e-6
DFF = 3584
NEG = -200.0
LROWS = 108
R_ADAB, R_MNG, R_FNG, R_CONVW, R_CONVB, R_BA, R_BX, R_LAM = 0, 48, 56, 64, 80, 84, 92, 100
R_FINALG, R_C, R_CC = 216, 224, 232
SCN = 15000
import os
ATT_PARTIAL = 0
MOE_SPARSE = int(os.environ.get('MOE_SPARSE', '1'))
I32 = mybir.dt.int32


def mkap(base, dims):
    return bass.AP(base.tensor, base.offset, [list(base.ap[0])] + [list(d) for d in dims])


def build_program(debug=None):
    nc = bass.Bass("TRN2", target_bir_lowering=False)

    def din(name, shape):
        return nc.dram_tensor(name, shape, F32, kind="ExternalInput").ap()

    x_d = din("x", [2048, 1024])
    ctx_d = din("ctx", [256, 1024])
    vecs_d = din("vecs", [256, 128])
    ada_w = din("ada_w", [2, 1024, 6144])
    w_in = din("w_in", [2, 1024, 2560])
    w_out = din("w_out", [2, 1024, 1024])
    ffn_w1 = din("ffn_w1", [1, 1024, DFF])
    ffn_w3 = din("ffn_w3", [1, 1024, DFF])
    ffn_w2 = din("ffn_w2", [1, DFF, 1024])
    moe_w1 = din("moe_w1", [1, 8, 1024, DFF])
    moe_w3 = din("moe_w3", [1, 8, 1024, DFF])
    moe_w2 = din("moe_w2", [1, 8, DFF, 1024])
    router_d = din("moe_router", [1, 1024, 8])
    rb_d = din("rb", [8, 1])
    wbd_d = din("wbd", [2, 2, 2, 4, 128, 128])
    tb_d = din("tb", [2, 4, 2, 2, 128, 1024])
    ident_d = din("ident", [128, 128])
    sel_d = din("sel", [8, 1024])
    ustr_d = din("ustrict", [128, 128])
    iota_d = din("iota", [128, 512])
    out_d = nc.dram_tensor("out", [2048, 1024], F32, kind="ExternalOutput").ap()
    dbg_d = None
    if debug is not None:
        dbg_d = nc.dram_tensor("dbg", [128, 8 * NT], F32, kind="ExternalOutput").ap()

    es = ExitStack()
    with es:
        def sb(name, shape, dt=F32):
            return es.enter_context(nc.sbuf_tensor('s_' + name, shape, dt))

        hT = sb("hT", [128, 8, NT])
        aT = sb("aT", [128, 8, NT], BF16)
        qo = sb("qo", [128, 4, NT], BF16)
        gro = sb("gro", [128, 4, NT], BF16)
        vT = sb("vT", [128, 256])
        ident = sb("ident", [128, 128])
        onesf = sb("onesf", [128, 128])
        cst = sb("cst", [128, 16])
        identb = sb("identb", [128, 128], BF16)
        flags_i = sb("flags_i", [128, 32], I32)
        mod = sb("mod", [128, 2, 48, 2])
        gsc = sb("gsc", [128, 2, 2, 8, 2])
        scb = sb("scb", [128, 8, 2], BF16)
        lruc = sb("lruc", [128, 2, 3, 8])
        router_sb = sb("router_sb", [128, 8, 8])
        rb_sb = sb("rb_sb", [8, 1])
        selb = [sb("selb0", [8, 128]), sb("selb1", [8, 128])]
        sc = sb("sc", [128, SCN])
        ps = es.enter_context(nc.psum_tensor("ps", [128, 4096], F32))

        def bank(i, n=512, off=0):
            return ps[:, i * 512 + off:i * 512 + off + n]

        def PB(i):
            return f"ps{i}"

        r = Rec()
        ctr = {'cp': 0}

        class Carver:
            def __init__(self):
                self.off = 0

            def f32(self, n):
                v = sc[:, self.off:self.off + n]
                self.off += n
                assert self.off <= SCN, self.off
                return v

            def bf16(self, n):
                assert n % 2 == 0
                v = sc[:, self.off:self.off + n // 2].bitcast(BF16)
                self.off += n // 2
                assert self.off <= SCN, self.off
                return v

        def copy_any(out, in_, reads, writes, eng=None):
            if eng is None:
                eng = 'act' if ctr['cp'] % 2 == 0 else 'dve'
                ctr['cp'] += 1
            if eng == 'act':
                r.op('act', [lambda e: e.activation(out=out, in_=in_, func=AF.Identity)], reads=reads, writes=writes)
            else:
                r.op(eng, [lambda e: e.tensor_copy(out=out, in_=in_)], reads=reads, writes=writes)

        qof_ = qo[:].rearrange("p c t -> p (c t)")
        grf_ = gro[:].rearrange("p c t -> p (c t)")

        def a2tok(t):
            return qof_[:, t * 1024:(t + 1) * 1024] if t < 9 else grf_[:, (t - 9) * 1024:(t - 8) * 1024]

        def vcol(row):
            return vT[:, row:row + 1]

        def blk_of(t):
            return min(t // 512, 4)

        cv = Carver()
        vst = cv.f32(256).rearrange("p (g n) -> p g n", g=2)
        xst = [cv.f32(1024) for _ in range(4)]
        r.op('sp', [lambda e: e.dma_start(out=ident[:], in_=ident_d)], writes=['ident'], dma='c0')
        r.op('sp', [lambda e: e.dma_start(out=vst, in_=vecs_d.rearrange("(g p) n -> p g n", p=128))], writes=['vst'], dma='c1')
        r.op('sp', [lambda e: e.dma_start(out=router_sb[:], in_=router_d[0].rearrange("(c p) n -> p c n", p=128))], writes=['router'], dma='c2')
        r.op('sp', [lambda e: e.dma_start(out=rb_sb[:], in_=rb_d)], writes=['rb'], dma='c3')
        r.op('pool', [lambda e: e.memset(onesf[:], 1.0 / 1024.0)], writes=['onesf'])
        r.op('pool', [lambda e: e.memset(cst[:, 0:1], EPS)], writes=['cst'])
        r.op('pool', [lambda e: e.memset(cst[:, 1:2], 1.0)], writes=['cst'])
        r.op('pool', [lambda e: e.memset(cst[:, 2:3], -1e30)], writes=['cst'])
        r.op('pool', [lambda e: e.memset(cst[:, 3:4], 0.0)], writes=['cst'])
        r.op('pool', [lambda e: e.memset(cst[:, 4:5], -1.0)], writes=['cst'])
        for b_ in range(4):
            r.op('pool', [lambda e, b_=b_: e.memset(cst[:, 5 + b_:6 + b_], 512.0 * b_ + 0.5)], writes=['cst'])
            r.op('pool', [lambda e, b_=b_: e.memset(cst[:, 9 + b_:10 + b_], -512.0 * b_)], writes=['cst'])
        r.op('dve', [lambda e: e.tensor_copy(out=identb[:], in_=ident[:])], reads=['ident'], writes=['identb'])
        for g in range(2):
            r.op('pe', [lambda e, g=g: e.transpose(out=bank(0, 128, g * 128), in_=vst[:, g, :], identity=ident[:])],
                 reads=['vst', 'ident'], writes=[PB(0)])
        r.op('dve', [lambda e: e.tensor_copy(out=vT[:], in_=bank(0, 256))], writes=[PB(0), 'vT'])
        r.op('act', [lambda e: e.activation(out=scb[:, :, 0], in_=vT[:, R_C:R_C + 8], func=AF.Silu)], reads=['vT'], writes=['scb0'])
        r.op('act', [lambda e: e.activation(out=scb[:, :, 1], in_=vT[:, R_CC:R_CC + 8], func=AF.Silu)], reads=['vT'], writes=['scb1'])
        for l in range(2):
            lam = vT[:, l * LROWS + R_LAM:l * LROWS + R_LAM + 8]
            r.op('act', [lambda e, l=l, lam=lam: e.activation(out=lruc[:, l, 2, :], in_=lam, func=AF.Exp, scale=-1.0)], reads=['vT'], writes=[f'lruc{l}'])
            r.op('act', [lambda e, l=l: e.activation(out=lruc[:, l, 2, :], in_=lruc[:, l, 2, :], func=AF.Ln, bias=cst[:, 1:2])], reads=['cst'], writes=[f'lruc{l}'])
            r.op('act', [lambda e, l=l: e.activation(out=lruc[:, l, 0, :], in_=lruc[:, l, 2, :], func=AF.Identity, scale=-8.0)], writes=[f'lruc{l}'])
            r.op('act', [lambda e, l=l: e.activation(out=lruc[:, l, 1, :], in_=lruc[:, l, 2, :], func=AF.Identity, scale=-16.0)], writes=[f'lruc{l}'])

        for tile in range(18):
            src = x_d[tile * 128:(tile + 1) * 128, :] if tile < 16 else ctx_d[(tile - 16) * 128:(tile - 15) * 128, :]
            xs = xst[tile % 4]
            r.op('sp', [lambda e, xs=xs, src=src: e.dma_start(out=xs, in_=src)], writes=[f'xst{tile % 4}'], dma=f'x{tile % 4}')
            bk = (tile % 2) * 2 + 2
            for half in range(2):
                r.op('pe', [lambda e, xs=xs, c=c, bk=bk, half=half: e.transpose(
                    out=bank(bk + half, 128, (c % 4) * 128), in_=xs[:, c * 128:(c + 1) * 128], identity=ident[:])
                    for c in range(half * 4, half * 4 + 4)],
                    reads=[f'xst{tile % 4}', 'ident'], writes=[PB(bk + half)])
                dst = hT[:, half * 4:half * 4 + 4, tile * 128:(tile + 1) * 128]
                srcp = bank(bk + half).rearrange("p (c n) -> p c n", c=4)
                copy_any(dst, srcp, [], [PB(bk + half)] + [f'hT:{c}:{blk_of(tile * 128)}' for c in range(half * 4, half * 4 + 4)])

        def dump_hT_and_finish():
            for c in range(8):
                r.op('sp', [lambda e, c=c: e.dma_start(out=dbg_d[:, c * NT:(c + 1) * NT], in_=hT[:, c, :])],
                     reads=[f'hT:{c}:{b}' for b in range(5)], writes=['dbg'], dma='dbg')
            r.op('sp', [], reads=['dbg'], final=True)

        def dump_bf16(t3, nchunks, resnames):
            r.barrier()
            cvd = Carver()
            stg = cvd.f32(NT)
            for c in range(nchunks):
                r.op('dve', [lambda e, c=c: e.tensor_copy(out=stg, in_=t3[:, c, :])], reads=resnames(c), writes=['stg'])
                r.op('sp', [lambda e, c=c: e.dma_start(out=dbg_d[:, c * NT:(c + 1) * NT], in_=stg)], reads=['stg'], writes=['dbg'], dma='dbg')
            r.op('sp', [], reads=['dbg'], final=True)

        def adaln(l):
            cva = Carver()
            cva.off = 4352
            wA = [cva.bf16(8 * 1024).rearrange("p (k n) -> p k n", k=8) for _ in range(2)]

            def load(g):
                buf = wA[g % 2]
                r.op('pool', [lambda e: e.dma_start(out=buf, in_=ada_w[l][:, g * 1024:(g + 1) * 1024].rearrange("(k p) n -> p k n", p=128))],
                     writes=[f'wA{g % 2}'], dma=f'wA{g % 2}')

            load(0)
            for g in range(6):
                if g + 1 < 6:
                    load(g + 1)
                buf = wA[g % 2]
                pbk = g % 2
                for j in range(8):
                    r.op('pe', [lambda e, buf=buf, j=j, k=k, pbk=pbk: e.matmul(bank(pbk, 2, j * 2), buf[:, k, j * 128:(j + 1) * 128], scb[:, k, :],
                                                                       start=(k == 0), stop=(k == 7)) for k in range(8)],
                         reads=[f'wA{g % 2}', 'scb0', 'scb1'], writes=[PB(pbk)])
                psv = bank(pbk, 16).rearrange("p (j t) -> p j t", t=2)
                for col in range(2):
                    r.op('dve', [lambda e, psv=psv, col=col, g=g: e.tensor_tensor(
                        out=mod[:, l, g * 8:(g + 1) * 8, col], in0=psv[:, :, col],
                        in1=vT[:, l * LROWS + R_ADAB + g * 8:l * LROWS + R_ADAB + g * 8 + 8], op=ALU.add)],
                        reads=['vT'], writes=[PB(pbk), f'mod{l}'])
            for n, (grow, scw) in enumerate(((R_MNG, 1), (R_FNG, 4))):
                for col in range(2):
                    r.op('dve', [lambda e, n=n, grow=grow, scw=scw, col=col: e.scalar_tensor_tensor(
                        out=gsc[:, l, n, :, col], in0=mod[:, l, scw * 8:scw * 8 + 8, col], scalar=cst[:, 1:2],
                        in1=vT[:, l * LROWS + grow:l * LROWS + grow + 8], op0=ALU.add, op1=ALU.mult)],
                        reads=[f'mod{l}', 'vT', 'cst'], writes=[f'gsc{l}'])

        def norm_mod(l, n, blocks, cvx, router=False, logitsT=None):
            sq = [cvx.f32(512) for _ in range(2)]
            lnv = cvx.f32(512)
            rstd = [cvx.f32(512) for _ in range(2)]
            tm = [cvx.f32(512) for _ in range(2)]
            a2f = [cvx.f32(512) for _ in range(2)] if router else None
            shw = 0 if n == 0 else 3
            for bi in blocks:
                t0, sz = BLKS[bi]
                col = 1 if bi == 4 else 0
                pbk = bi % 2
                for c in range(8):
                    s = sq[c % 2]
                    r.op('act', [lambda e, s=s, c=c, t0=t0, sz=sz: e.activation(out=s[:, :sz], in_=hT[:, c, t0:t0 + sz], func=AF.Square)],
                         reads=[f'hT:{c}:{bi}'], writes=[f'sq{c % 2}'])
                    r.op('pe', [lambda e, s=s, c=c, pbk=pbk, sz=sz: e.matmul(bank(pbk, sz), onesf[:], s[:, :sz], start=(c == 0), stop=(c == 7))],
                         reads=[f'sq{c % 2}', 'onesf'], writes=[PB(pbk)])
                rs = rstd[bi % 2]
                r.op('act', [lambda e, pbk=pbk, sz=sz: e.activation(out=lnv[:, :sz], in_=bank(pbk, sz), func=AF.Ln, bias=cst[:, 0:1])],
                     reads=['cst'], writes=[PB(pbk), 'lnv'])
                r.op('act', [lambda e, rs=rs, sz=sz: e.activation(out=rs[:, :sz], in_=lnv[:, :sz], func=AF.Exp, scale=-0.5)],
                     reads=['lnv'], writes=[f'rstd{bi % 2}'])
                for c in range(8):
                    t = tm[c % 2]
                    r.op('dve', [lambda e, t=t, c=c, rs=rs, t0=t0, sz=sz: e.tensor_tensor(out=t[:, :sz], in0=hT[:, c, t0:t0 + sz], in1=rs[:, :sz], op=ALU.mult)],
                         reads=[f'hT:{c}:{bi}', f'rstd{bi % 2}'], writes=[f'tm{c % 2}'])
                    bias_ap = mod[:, l, shw * 8 + c, col:col + 1]
                    scale_ap = gsc[:, l, n, c, col:col + 1]
                    if not router:
                        r.op('act', [lambda e, t=t, c=c, t0=t0, sz=sz, bias_ap=bias_ap, scale_ap=scale_ap: e.activation(
                            out=aT[:, c, t0:t0 + sz], in_=t[:, :sz], func=AF.Identity, bias=bias_ap, scale=scale_ap)],
                            reads=[f'tm{c % 2}', f'mod{l}', f'gsc{l}'], writes=[f'aT:{c}:{bi}'])
                    else:
                        af = a2f[c % 2]
                        r.op('act', [lambda e, t=t, af=af, sz=sz, bias_ap=bias_ap, scale_ap=scale_ap: e.activation(
                            out=af[:, :sz], in_=t[:, :sz], func=AF.Identity, bias=bias_ap, scale=scale_ap)],
                            reads=[f'tm{c % 2}', f'mod{l}', f'gsc{l}'], writes=[f'a2f{c % 2}'])
                        if not MOE_SPARSE:
                            r.op('dve', [lambda e, c=c, af=af, t0=t0, sz=sz: e.tensor_copy(out=aT[:, c, t0:t0 + sz], in_=af[:, :sz])],
                                 reads=[f'a2f{c % 2}'], writes=[f'aT:{c}:{bi}'])
                        else:
                            pbt = 4 + c % 2
                            r.op('pe', [lambda e, af=af, tt=tt, pbt=pbt: e.transpose(out=bank(pbt, 128, tt * 128), in_=af[:, tt * 128:(tt + 1) * 128], identity=ident[:])
                                        for tt in range(sz // 128)], reads=[f'a2f{c % 2}', 'ident'], writes=[PB(pbt)])
                            for tt in range(sz // 128):
                                tk = t0 // 128 + tt
                                copy_any(a2tok(tk)[:, c * 128:(c + 1) * 128], bank(pbt, 128, tt * 128), [], [PB(pbt), f'a2tok{tk}'])
                        r.op('pe', [lambda e, c=c, af=af, bi=bi, sz=sz: e.matmul(bank(2 + bi % 2, sz)[0:8, :], router_sb[:, c, :], af[:, :sz], start=(c == 0), stop=(c == 7))],
                             reads=[f'a2f{c % 2}', 'router'], writes=[PB(2 + bi % 2)])
                if router:
                    r.op('act', [lambda e, bi=bi, t0=t0, sz=sz: e.activation(out=logitsT[0:8, t0:t0 + sz], in_=bank(2 + bi % 2, sz)[0:8, :],
                                                                           func=AF.Identity, bias=rb_sb[0:8, 0:1])],
                         reads=['rb'], writes=[PB(2 + bi % 2), 'logitsT'])

        def lru_phase(l, last):
            cvl = Carver()
            wx = [cvl.bf16(8 * 128).rearrange("p (k n) -> p k n", k=8) for _ in range(2)]
            wg = [cvl.bf16(8 * 128).rearrange("p (k n) -> p k n", k=8) for _ in range(2)]
            wb = [cvl.bf16(4 * 128).rearrange("p (d a n) -> p d a n", d=2, a=2) for _ in range(2)]
            xp = cvl.f32(2312)
            u = cvl.f32(NT)
            ub = cvl.bf16(NT)
            tr_ = [cvl.f32(512) for _ in range(3)]
            ti_ = [cvl.f32(512) for _ in range(3)]
            ta_ = [cvl.f32(512) for _ in range(2)]
            tm_ = [cvl.f32(512) for _ in range(2)]
            thr = [cvl.f32(512) for _ in range(2)]
            hf = xp[:, 0:NT]
            base = l * LROWS

            def load(c):
                b = c % 2
                r.op('pool', [lambda e: e.dma_start(out=wx[b], in_=w_in[l][:, 1536 + c * 128:1536 + (c + 1) * 128].rearrange("(k p) n -> p k n", p=128))],
                     writes=[f'wx{b}'], dma=f'wx{b}')
                r.op('pool', [lambda e: e.dma_start(out=wg[b], in_=w_in[l][:, 2048 + c * 128:2048 + (c + 1) * 128].rearrange("(k p) n -> p k n", p=128))],
                     writes=[f'wg{b}'], dma=f'wg{b}')
                r.op('pool', [lambda e: e.dma_start(out=wb[b], in_=wbd_d[l, :, :, c].rearrange("d a p n -> p d a n"))],
                     writes=[f'wb{b}'], dma=f'wb{b}')

            load(0)
            it = 0
            for c in range(4):
                if c + 1 < 4:
                    load(c + 1)
                b = c % 2
                for (a0, a1) in ((0, 1), (2049, 2052), (2308, 2312)):
                    r.op('pool', [lambda e, a0=a0, a1=a1: e.memset(xp[:, a0:a1], 0.0)], writes=['xp'])
                for bi in range(5):
                    t0, sz = BLKS[bi]
                    xo = t0 + 1 if bi < 4 else t0 + 4
                    pa, pg = 4 + bi % 2, 6 + bi % 2
                    r.op('pe', [lambda e, k=k, pa=pa, b=b, t0=t0, sz=sz: e.matmul(bank(pa, sz), wx[b][:, k, :], aT[:, k, t0:t0 + sz], start=(k == 0), stop=(k == 7)) for k in range(8)],
                         reads=[f'wx{b}'] + [f'aT:{k}:{bi}' for k in range(8)], writes=[PB(pa)])
                    r.op('act', [lambda e, pa=pa, xo=xo, sz=sz: e.activation(out=xp[:, xo:xo + sz], in_=bank(pa, sz), func=AF.Identity)],
                         writes=[PB(pa), 'xp'])
                    r.op('pe', [lambda e, k=k, pg=pg, b=b, t0=t0, sz=sz: e.matmul(bank(pg, sz), wg[b][:, k, :], aT[:, k, t0:t0 + sz], start=(k == 0), stop=(k == 7)) for k in range(8)],
                         reads=[f'wg{b}'] + [f'aT:{k}:{bi}' for k in range(8)], writes=[PB(pg)])
                    r.op('act', [lambda e, pg=pg, c=c, t0=t0, sz=sz: e.activation(out=gro[:, c, t0:t0 + sz], in_=bank(pg, sz), func=AF.Gelu_apprx_tanh)],
                         writes=[PB(pg), f'gro:{c}:{bi}'])
                for (o0, n0, x0) in ((0, NL, 0), (NL, 256, 2051)):
                    r.op('dve', [lambda e, o0=o0, n0=n0, x0=x0, c=c: e.tensor_scalar(
                        out=u[:, o0:o0 + n0], in0=xp[:, x0:x0 + n0], scalar1=vcol(base + R_CONVW + 0 * 4 + c), scalar2=vcol(base + R_CONVB + c),
                        op0=ALU.mult, op1=ALU.add)], reads=['xp', 'vT'], writes=['u'])
                    for j in range(1, 4):
                        r.op('dve', [lambda e, o0=o0, n0=n0, x0=x0, j=j, c=c: e.scalar_tensor_tensor(
                            out=u[:, o0:o0 + n0], in0=xp[:, x0 + j:x0 + j + n0], scalar=vcol(base + R_CONVW + j * 4 + c), in1=u[:, o0:o0 + n0],
                            op0=ALU.mult, op1=ALU.add)], reads=['xp', 'vT'], writes=['u'])
                r.op('act', [lambda e: e.activation(out=ub, in_=u, func=AF.Identity)], reads=['u'], writes=['ub'])
                for d in range(2):
                    order = [4, 0, 1, 2, 3] if d == 0 else [4, 3, 2, 1, 0]
                    prev = None
                    prev_k2 = None
                    ba_ap = vcol(base + R_BA + d * 4 + c)
                    bx_ap = vcol(base + R_BX + d * 4 + c)
                    nsp1 = lruc[:, l, 0, d * 4 + c:d * 4 + c + 1]
                    nsp2 = lruc[:, l, 1, d * 4 + c:d * 4 + c + 1]
                    for grp in (order[0:2], order[2:4], order[4:5]):
                        infos = []
                        for bi in grp:
                            t0, sz = BLKS[bi]
                            k2 = it % 2
                            k3 = it % 3
                            it += 1
                            pr, pi = 4 + k2, 6 + k2
                            tr, ti = tr_[k3], ti_[k3]
                            r.op('pe', [lambda e, pr=pr, b=b, d=d, t0=t0, sz=sz: e.matmul(bank(pr, sz), wb[b][:, d, 0, :], ub[:, t0:t0 + sz], start=True, stop=True)],
                                 reads=[f'wb{b}', 'ub'], writes=[PB(pr)])
                            r.op('pe', [lambda e, pi=pi, b=b, d=d, t0=t0, sz=sz: e.matmul(bank(pi, sz), wb[b][:, d, 1, :], ub[:, t0:t0 + sz], start=True, stop=True)],
                                 reads=[f'wb{b}', 'ub'], writes=[PB(pi)])
                            r.op('act', [lambda e, pr=pr, tr=tr, sz=sz, ba_ap=ba_ap: e.activation(out=tr[:, :sz], in_=bank(pr, sz), func=AF.Sigmoid, bias=ba_ap)],
                                 reads=['vT'], writes=[PB(pr), f'tr{k3}'])
                            r.op('act', [lambda e, pi=pi, ti=ti, sz=sz, bx_ap=bx_ap: e.activation(out=ti[:, :sz], in_=bank(pi, sz), func=AF.Sigmoid, bias=bx_ap)],
                                 reads=['vT'], writes=[PB(pi), f'ti{k3}'])
                            infos.append((bi, t0, sz, k2, k3))
                        for (bi, t0, sz, k2, k3) in infos:
                            tr, ta, tmm = tr_[k3], ta_[k2], tm_[k2]
                            r.op('act', [lambda e, tr=tr, ta=ta, sz=sz, nsp1=nsp1: e.activation(out=ta[:, :sz], in_=tr[:, :sz], func=AF.Exp, scale=nsp1)],
                                 reads=[f'tr{k3}', f'lruc{l}'], writes=[f'ta{k2}'])
                            r.op('act', [lambda e, tr=tr, tmm=tmm, sz=sz, nsp2=nsp2: e.activation(out=tmm[:, :sz], in_=tr[:, :sz], func=AF.Exp, scale=nsp2)],
                                 reads=[f'tr{k3}', f'lruc{l}'], writes=[f'tm{k2}'])
                            r.op('act', [lambda e, tmm=tmm, sz=sz: e.activation(out=tmm[:, :sz], in_=tmm[:, :sz], func=AF.Ln, scale=-1.0, bias=cst[:, 1:2])],
                                 reads=['cst'], writes=[f'tm{k2}'])
                            r.op('act', [lambda e, tmm=tmm, sz=sz: e.activation(out=tmm[:, :sz], in_=tmm[:, :sz], func=AF.Exp, scale=0.5)],
                                 writes=[f'tm{k2}'])
                        for (bi, t0, sz, k2, k3) in infos:
                            ti, ta, tmm, hr = ti_[k3], ta_[k2], tm_[k2], thr[k2]
                            r.op('dve', [lambda e, ti=ti, tmm=tmm, sz=sz: e.tensor_tensor(out=ti[:, :sz], in0=ti[:, :sz], in1=tmm[:, :sz], op=ALU.mult)],
                                 reads=[f'tm{k2}'], writes=[f'ti{k3}'])
                            r.op('dve', [lambda e, ti=ti, t0=t0, sz=sz: e.tensor_tensor(out=ti[:, :sz], in0=ti[:, :sz], in1=u[:, t0:t0 + sz], op=ALU.mult)],
                                 reads=['u'], writes=[f'ti{k3}'])
                            if d == 0:
                                if prev is None:
                                    init = 0.0
                                else:
                                    pt0, psz = BLKS[prev]
                                    init = hf[:, pt0 + psz - 1:pt0 + psz]
                                r.op('dve', [lambda e, ta=ta, ti=ti, init=init, t0=t0, sz=sz: e.tensor_tensor_scan(
                                    out=hf[:, t0:t0 + sz], data0=ta[:, :sz], data1=ti[:, :sz], initial=init, op0=ALU.mult, op1=ALU.add)],
                                    reads=[f'ta{k2}', f'ti{k3}'], writes=['xp'])
                            else:
                                init = 0.0 if prev is None else thr[prev_k2][:, 0:1]
                                rd = [f'ta{k2}', f'ti{k3}'] + ([] if prev is None else [f'hr{prev_k2}'])
                                r.op('dve', [lambda e, ta=ta, ti=ti, hr=hr, init=init, sz=sz: e.tensor_tensor_scan(
                                    out=mkap(hr[:, sz - 1:sz], [[-1, sz]]), data0=mkap(ta[:, sz - 1:sz], [[-1, sz]]),
                                    data1=mkap(ti[:, sz - 1:sz], [[-1, sz]]), initial=init, op0=ALU.mult, op1=ALU.add)],
                                    reads=rd, writes=[f'hr{k2}'])
                                if not (last and bi == 4):
                                    r.op('dve', [lambda e, hr=hr, tmm=tmm, t0=t0, sz=sz: e.tensor_tensor(out=tmm[:, :sz], in0=hr[:, :sz], in1=hf[:, t0:t0 + sz], op=ALU.add)],
                                         reads=[f'hr{k2}', 'xp'], writes=[f'tm{k2}'])
                                    r.op('dve', [lambda e, tmm=tmm, c=c, t0=t0, sz=sz: e.tensor_tensor(out=gro[:, c, t0:t0 + sz], in0=gro[:, c, t0:t0 + sz], in1=tmm[:, :sz], op=ALU.mult)],
                                         reads=[f'tm{k2}'], writes=[f'gro:{c}:{bi}'])
                            prev = bi
                            prev_k2 = k2

        def attn_phase(l, last):
            cva = Carver()
            wq = [cva.bf16(1024).rearrange("p (k n) -> p k n", k=8)] * 2
            wk = [cva.bf16(1024).rearrange("p (k n) -> p k n", k=8)] * 2
            wv = [cva.bf16(1024).rearrange("p (k n) -> p k n", k=8)] * 2
            kT = [cva.bf16(NT) for _ in range(2)]
            V = [cva.bf16(18 * 192).rearrange("p (t n) -> p t n", t=18) for _ in range(2)]
            T = [cva.bf16(4096).rearrange("p (v j n) -> p v j n", v=2, j=2) for _ in range(2)]
            Tb32 = [cva.f32(1024) for _ in range(2)]
            P = [cva.bf16(1024) for _ in range(2)]
            rec = [cva.f32(128) for _ in range(2)]
            otok = [cva.bf16(128) for _ in range(2)]
            for b in range(2):
                r.op('pool', [lambda e, b=b: e.memset(V[b][:, :, 64:128], 1.0)], writes=[f'V{b}'])

            def load(hp):
                for nm, buf, c0 in (('wq', wq, 0), ('wk', wk, 512), ('wv', wv, 1024)):
                    r.op('pool', [lambda e, buf=buf, c0=c0, hp=hp: e.dma_start(
                        out=buf[0], in_=w_in[l][:, c0 + hp * 128:c0 + (hp + 1) * 128].rearrange("(k p) n -> p k n", p=128))],
                        writes=[f'{nm}'], dma=f'{nm}')

            def load_T(hp):
                bq = hp % 2
                for v in range(2):
                    for j in range(2):
                        q = (v * 2 + j) % 2
                        r.op('sp', [lambda e, hp=hp, v=v, j=j, q=q: e.dma_start(out=Tb32[q], in_=tb_d[l, hp, v, j])], writes=[f'Tb32{q}'], dma=f'tb{q}')
                        r.op('act', [lambda e, bq=bq, v=v, j=j, q=q: e.activation(out=T[bq][:, v, j, :], in_=Tb32[q], func=AF.Exp)], reads=[f'Tb32{q}'], writes=[f'T{bq}'])

            load(0)
            load_T(0)
            qblocks = range(4) if last else range(5)
            npairs = 16 if last else 18
            itc = 0
            def project(hp):
                b = hp % 2
                for bi in qblocks:
                    t0, sz = BLKS[bi]
                    pb = 6 + bi % 2
                    r.op('pe', [lambda e, k=k, pb=pb, t0=t0, sz=sz, b=b: e.matmul(bank(pb, sz), wq[b][:, k, :], aT[:, k, t0:t0 + sz], start=(k == 0), stop=(k == 7)) for k in range(8)],
                         reads=['wq'] + [f'aT:{k}:{bi}' for k in range(8)], writes=[PB(pb)])
                    ms = range(t0 // 128, (t0 + sz) // 128)
                    copy_any(qo[:, hp, t0:t0 + sz], bank(pb, sz), [], [PB(pb)] + [f'qo:{hp}:{m}:{j}' for m in ms for j in range(2)])
                for bi in range(5):
                    t0, sz = BLKS[bi]
                    pb = 6 + (bi + 1) % 2
                    r.op('pe', [lambda e, k=k, pb=pb, t0=t0, sz=sz, b=b: e.matmul(bank(pb, sz), wk[b][:, k, :], aT[:, k, t0:t0 + sz], start=(k == 0), stop=(k == 7)) for k in range(8)],
                         reads=['wk'] + [f'aT:{k}:{bi}' for k in range(8)], writes=[PB(pb)])
                    copy_any(kT[b][:, t0:t0 + sz], bank(pb, sz), [], [PB(pb), f'kT{b}'])
                for tile in range(18):
                    pb = 6 + tile % 2
                    bi = blk_of(tile * 128)
                    r.op('pe', [lambda e, k=k, pb=pb, tile=tile, b=b: e.matmul(bank(pb, 128), aT[:, k, tile * 128:(tile + 1) * 128], wv[b][:, k, :], start=(k == 0), stop=(k == 7)) for k in range(8)],
                         reads=['wv'] + [f'aT:{k}:{bi}' for k in range(8)], writes=[PB(pb)])
                    dst = mkap(V[b][:, tile, 0:64], [[128, 2], [1, 64]])
                    srcv = bank(pb, 128).rearrange("p (a n) -> p a n", a=2)
                    copy_any(dst, srcv, [], [PB(pb), f'V{b}'])
            for hp in range(4):
                b = hp % 2
                if hp == 0:
                    project(0)
                if hp + 1 < 4:
                    load(hp + 1)
                    load_T(hp + 1)
                iters = []
                for m in range(npairs):
                    if m < 16:
                        if 2 <= m <= 13:
                            tiles = [m + 2, m + 1, m, m - 1, m - 2]
                        elif m < 2:
                            tiles = [3, 2, 1, 0]
                        else:
                            tiles = [15, 14, 13, 12]
                        nw = len(tiles)
                        idx0 = 7 - (2 * tiles[0] - 2 * m)
                        tiles = tiles + [16, 17]
                    else:
                        tiles, nw, idx0 = [16, 17], 0, 0
                    for j in range(2):
                        iters.append((m, j, tiles, nw, idx0))

                def stage_a(itn, m, j, tiles, nw, idx0):
                    k2 = itn % 2
                    nt = len(tiles)
                    q0 = m * 128
                    pS = [0 + 2 * k2, 1 + 2 * k2]
                    Pb = P[k2]
                    rq = f'qo:{hp}:{m}:{j}'
                    r.op('pe', [lambda e, i=i, tl=tl, k2=k2, j=j, b=b, hp=hp, q0=q0: e.matmul(
                        ps[:, k2 * 1024 + i * 128:k2 * 1024 + (i + 1) * 128], kT[b][j * 64:(j + 1) * 64, tl * 128:(tl + 1) * 128],
                        qo[j * 64:(j + 1) * 64, hp, q0:q0 + 128], start=True, stop=True) for i, tl in enumerate(tiles)],
                        reads=[f'kT{b}', rq], writes=[PB(pS[0]), PB(pS[1])])
                    n0 = min(nt, 4) * 128
                    r.op('act', [lambda e, k2=k2, Pb=Pb, n0=n0: e.activation(out=Pb[:, 0:n0], in_=ps[:, k2 * 1024:k2 * 1024 + n0], func=AF.Exp, scale=0.125)],
                         writes=[PB(pS[0]), f'P{k2}'])
                    if nt > 4:
                        n1 = nt * 128
                        r.op('act', [lambda e, k2=k2, Pb=Pb, n1=n1: e.activation(out=Pb[:, 512:n1], in_=ps[:, k2 * 1024 + 512:k2 * 1024 + n1], func=AF.Exp, scale=0.125)],
                             writes=[PB(pS[1]), f'P{k2}'])
                    if nw > 0:
                        r.op('dve', [lambda e, Pb=Pb, nw=nw, idx0=idx0, j=j, b=b, m=m: e.tensor_tensor(
                            out=Pb[:, 0:nw * 128], in0=Pb[:, 0:nw * 128], in1=T[b][:, (1 if 2 <= m <= 13 else 0), j, idx0 * 64:idx0 * 64 + nw * 128], op=ALU.mult)],
                            reads=[f'T{b}'], writes=[f'P{k2}'])
                        if False:
                            r.op('pool', [lambda e, Pb=Pb: e.memset(Pb[0:64, 0:64], 0.0)], writes=[f'P{k2}'])
                            r.op('pool', [lambda e, Pb=Pb: e.memset(Pb[64:128, 0:128], 0.0)], writes=[f'P{k2}'])
                            r.op('pool', [lambda e, Pb=Pb: e.memset(Pb[0:64, 4 * 128 + 64:5 * 128], 0.0)], writes=[f'P{k2}'])

                def stage_b(itn, m, j, tiles, nw, idx0):
                    k2 = itn % 2
                    nt = len(tiles)
                    q0 = m * 128
                    pO = 4 + k2
                    Pb = P[k2]
                    rq = f'qo:{hp}:{m}:{j}'
                    c0, c1 = (0, 128) if j == 0 else (64, 192)
                    r.op('pe', [lambda e, i=i, tl=tl, Pb=Pb, pO=pO, b=b, nt=nt, c0=c0, c1=c1: e.matmul(
                        bank(pO, 128), V[b][:, tl, c0:c1], Pb[:, i * 128:(i + 1) * 128], start=(i == 0), stop=(i == nt - 1)) for i, tl in enumerate(tiles)],
                        reads=[f'V{b}', f'P{k2}'], writes=[PB(pO)])
                    rc = rec[k2]
                    if j == 0:
                        num, den, rcs = bank(pO, 128)[0:64, :], bank(pO, 128)[64:128, :], rc[0:64, :]
                    else:
                        num, den, rcs = bank(pO, 128)[64:128, :], bank(pO, 128)[0:64, :], rc[64:128, :]
                    r.op('dve', [lambda e, den=den, rcs=rcs: e.reciprocal(out=rcs, in_=den)], writes=[PB(pO), f'rec{k2}'])
                    r.op('dve', [lambda e, num=num, rcs=rcs, j=j, hp=hp, q0=q0: e.tensor_tensor(out=qo[j * 64:(j + 1) * 64, hp, q0:q0 + 128], in0=num, in1=rcs, op=ALU.mult)],
                         reads=[f'rec{k2}'], writes=[PB(pO), rq])

                stage_a(itc, *iters[0])
                for ii in range(len(iters)):
                    if ii + 1 < len(iters):
                        stage_a(itc + ii + 1, *iters[ii + 1])
                    stage_b(itc + ii, *iters[ii])
                    if ii == len(iters) // 3 and hp + 1 < 4:
                        project(hp + 1)
                itc += len(iters)

        def wout_phase(l, last):
            cvw = Carver()
            wo = cvw.bf16(8 * 1024).rearrange("p (k n) -> p k n", k=8)
            r.op('pool', [lambda e: e.dma_start(out=wo, in_=w_out[l].rearrange("(k p) n -> p k n", p=128))], writes=['wo'], dma='wo')
            it = 0
            for bi in (range(4) if last else range(5)):
                t0, sz = BLKS[bi]
                col = 1 if bi == 4 else 0
                ms = range(t0 // 128, (t0 + sz) // 128)
                rds = ['wo'] + [f'qo:{hp}:{m}:{j}' for hp in range(4) for m in ms for j in range(2)] + [f'gro:{c}:{bi}' for c in range(4)]
                for dc in range(8):
                    pb = it % 8
                    it += 1
                    r.op('pe', [lambda e, k=k, pb=pb, dc=dc, t0=t0, sz=sz: e.matmul(bank(pb, sz), wo[:, k, dc * 128:(dc + 1) * 128],
                                                                                    (qo[:, k, t0:t0 + sz] if k < 4 else gro[:, k - 4, t0:t0 + sz]),
                                                                                    start=(k == 0), stop=(k == 7)) for k in range(8)],
                         reads=rds, writes=[PB(pb)])
                    g1 = mod[:, l, 16 + dc, col:col + 1]
                    r.op('dve', [lambda e, pb=pb, dc=dc, t0=t0, sz=sz, g1=g1: e.scalar_tensor_tensor(
                        out=hT[:, dc, t0:t0 + sz], in0=bank(pb, sz), scalar=g1, in1=hT[:, dc, t0:t0 + sz],
                        op0=ALU.mult, op1=ALU.add)], reads=[f'mod{l}'], writes=[PB(pb), f'hT:{dc}:{bi}'])

        def ffn_phase(l, moe, blocks, cvf, gatesT=None):
            qof = qo[:].rearrange("p c t -> p (c t)")
            grf = gro[:].rearrange("p c t -> p (c t)")
            W1 = [qof[:, 0:4096].rearrange("p (k n) -> p k n", k=8), cvf.bf16(4096).rearrange("p (k n) -> p k n", k=8)]
            W3 = [qof[:, 4096:8192].rearrange("p (k n) -> p k n", k=8), cvf.bf16(4096).rearrange("p (k n) -> p k n", k=8)]
            W2 = [grf[:, 0:4096].rearrange("p (f n) -> p f n", f=4), cvf.bf16(4096).rearrange("p (f n) -> p f n", f=4)]
            G = [grf[:, 4096:6144].rearrange("p (f n) -> p f n", f=4), grf[:, 6144:8192].rearrange("p (f n) -> p f n", f=4)]
            S = [cvf.f32(512) for _ in range(2)]
            Tt = [cvf.f32(512) for _ in range(2)]
            gbc = [cvf.f32(NL) for _ in range(2)] if moe else None
            NE = 8 if moe else 1
            NG = DFF // 512
            seq = [(e_, g) for e_ in range(NE) for g in range(NG)]

            def load(i):
                e_, g = seq[i]
                b = i % 2
                w1s = (moe_w1[0, e_] if moe else ffn_w1[0])[:, g * 512:(g + 1) * 512].rearrange("(k p) n -> p k n", p=128)
                w3s = (moe_w3[0, e_] if moe else ffn_w3[0])[:, g * 512:(g + 1) * 512].rearrange("(k p) n -> p k n", p=128)
                w2s = (moe_w2[0, e_] if moe else ffn_w2[0])[g * 512:(g + 1) * 512, :].rearrange("(f p) n -> p f n", p=128)
                r.op('pool', [lambda e: e.dma_start(out=W1[b], in_=w1s)], writes=[f'W1{b}'], dma=f'W1{b}')
                r.op('pool', [lambda e: e.dma_start(out=W3[b], in_=w3s)], writes=[f'W3{b}'], dma=f'W3{b}')
                r.op('pool', [lambda e: e.dma_start(out=W2[b], in_=w2s)], writes=[f'W2{b}'], dma=f'W2{b}')

            load(0)
            ith = 0
            ito = 0
            itg = 0
            for i, (e_, g) in enumerate(seq):
                if i + 1 < len(seq):
                    load(i + 1)
                b = i % 2
                if moe and g == 0:
                    gb = gbc[e_ % 2]
                    sl = selb[e_ % 2]
                    r.op('sp', [lambda e, sl=sl, e_=e_: e.dma_start(out=sl[:], in_=sel_d[:, e_ * 128:(e_ + 1) * 128])], writes=[f'sel{e_ % 2}'], dma=f'sel{e_ % 2}')
                    for bi in blocks:
                        t0, sz = BLKS[bi]
                        pb = 4 + ito % 4
                        ito += 1
                        r.op('pe', [lambda e, pb=pb, t0=t0, sz=sz, sl=sl: e.matmul(bank(pb, sz), sl[0:8, :], gatesT[0:8, t0:t0 + sz], start=True, stop=True)],
                             reads=[f'sel{e_ % 2}', 'gatesT'], writes=[PB(pb)])
                        r.op('act', [lambda e, pb=pb, gb=gb, t0=t0, sz=sz: e.activation(out=gb[:, t0:t0 + sz], in_=bank(pb, sz), func=AF.Identity)],
                             writes=[PB(pb), f'gbc{e_ % 2}:{bi}'])
                for bi in blocks:
                    t0, sz = BLKS[bi]
                    col = 1 if bi == 4 else 0
                    Gb = G[itg % 2]
                    gname = f'G{itg % 2}'
                    itg += 1
                    for fc in range(4):
                        k2 = ith % 2
                        ith += 1
                        p1, p3 = k2, 2 + k2
                        r.op('pe', [lambda e, k=k, p1=p1, fc=fc, b=b, t0=t0, sz=sz: e.matmul(bank(p1, sz), W1[b][:, k, fc * 128:(fc + 1) * 128], aT[:, k, t0:t0 + sz],
                                                                                             start=(k == 0), stop=(k == 7)) for k in range(8)],
                             reads=[f'W1{b}'] + [f'aT:{k}:{bi}' for k in range(8)], writes=[PB(p1)])
                        r.op('pe', [lambda e, k=k, p3=p3, fc=fc, b=b, t0=t0, sz=sz: e.matmul(bank(p3, sz), W3[b][:, k, fc * 128:(fc + 1) * 128], aT[:, k, t0:t0 + sz],
                                                                                             start=(k == 0), stop=(k == 7)) for k in range(8)],
                             reads=[f'W3{b}'] + [f'aT:{k}:{bi}' for k in range(8)], writes=[PB(p3)])
                        s = S[k2]
                        r.op('act', [lambda e, p1=p1, s=s, sz=sz: e.activation(out=s[:, :sz], in_=bank(p1, sz), func=AF.Silu)], writes=[PB(p1), f'S{k2}'])
                        if moe:
                            tt = Tt[k2]
                            gbs = gbc[e_ % 2]
                            r.op('dve', [lambda e, p3=p3, tt=tt, gbs=gbs, t0=t0, sz=sz: e.tensor_tensor(out=tt[:, :sz], in0=bank(p3, sz), in1=gbs[:, t0:t0 + sz], op=ALU.mult)],
                                 reads=[f'gbc{e_ % 2}:{bi}'], writes=[PB(p3), f'Tt{k2}'])
                            r.op('dve', [lambda e, tt=tt, s=s, fc=fc, Gb=Gb, sz=sz: e.tensor_tensor(out=Gb[:, fc, :sz], in0=tt[:, :sz], in1=s[:, :sz], op=ALU.mult)],
                                 reads=[f'Tt{k2}', f'S{k2}'], writes=[gname])
                        else:
                            r.op('dve', [lambda e, p3=p3, s=s, fc=fc, Gb=Gb, sz=sz: e.tensor_tensor(out=Gb[:, fc, :sz], in0=bank(p3, sz), in1=s[:, :sz], op=ALU.mult)],
                                 reads=[f'S{k2}'], writes=[PB(p3), gname])
                    for dc in range(8):
                        pb = 4 + ito % 4
                        ito += 1
                        r.op('pe', [lambda e, fc=fc, pb=pb, dc=dc, Gb=Gb, b=b, sz=sz: e.matmul(bank(pb, sz), W2[b][:, fc, dc * 128:(dc + 1) * 128], Gb[:, fc, :sz],
                                                                                               start=(fc == 0), stop=(fc == 3)) for fc in range(4)],
                             reads=[f'W2{b}', gname], writes=[PB(pb)])
                        g2 = mod[:, l, 40 + dc, col:col + 1]
                        r.op('dve', [lambda e, pb=pb, dc=dc, t0=t0, sz=sz, g2=g2: e.scalar_tensor_tensor(
                            out=hT[:, dc, t0:t0 + sz], in0=bank(pb, sz), scalar=g2, in1=hT[:, dc, t0:t0 + sz],
                            op0=ALU.mult, op1=ALU.add)], reads=[f'mod{l}'], writes=[PB(pb), f'hT:{dc}:{bi}'])

        def moe_gates(cvm, logitsT, gatesT):
            lg = cvm.f32(128).rearrange("p (t e) -> p t e", e=8)
            l2 = cvm.f32(128).rearrange("p (t e) -> p t e", e=8)
            eq1 = cvm.f32(128).rearrange("p (t e) -> p t e", e=8)
            eq2 = cvm.f32(128).rearrange("p (t e) -> p t e", e=8)
            gt = cvm.f32(128).rearrange("p (t e) -> p t e", e=8)
            m1 = cvm.f32(16)
            m2 = cvm.f32(16)
            w1 = cvm.f32(16)
            w2 = cvm.f32(16)

            def bc(v):
                return mkap(v[:, 0:1], [[1, 16], [0, 8]])

            r.op('pe', [lambda e, t=t: e.transpose(out=bank(0, 8, t * 8), in_=logitsT[0:8, t * 128:(t + 1) * 128], identity=ident[0:8, 0:8]) for t in range(16)],
                 reads=['logitsT', 'ident'], writes=[PB(0)])
            r.op('dve', [lambda e: e.tensor_copy(out=lg, in_=bank(0, 128).rearrange("p (t e) -> p t e", e=8))], writes=[PB(0), 'lg'])
            AX = mybir.AxisListType.X
            r.op('dve', [lambda e: e.tensor_reduce(out=m1, in_=lg, axis=AX, op=ALU.max)], writes=['lg', 'm1'])
            r.op('dve', [lambda e: e.tensor_tensor(out=eq1, in0=lg, in1=bc(m1), op=ALU.is_equal)], writes=['lg', 'm1', 'eq1'])
            r.op('dve', [lambda e: e.scalar_tensor_tensor(out=l2, in0=eq1, scalar=cst[:, 2:3], in1=lg, op0=ALU.mult, op1=ALU.add)], reads=['cst'], writes=['eq1', 'lg', 'l2'])
            r.op('dve', [lambda e: e.tensor_reduce(out=m2, in_=l2, axis=AX, op=ALU.max)], writes=['l2', 'm2'])
            r.op('dve', [lambda e: e.tensor_tensor(out=eq2, in0=l2, in1=bc(m2), op=ALU.is_equal)], writes=['l2', 'm2', 'eq2'])
            r.op('dve', [lambda e: e.tensor_tensor(out=w2, in0=m2, in1=m1, op=ALU.subtract)], writes=['m1', 'm2', 'w2'])
            r.op('act', [lambda e: e.activation(out=w2, in_=w2, func=AF.Exp)], writes=['w2'])
            r.op('act', [lambda e: e.activation(out=w1, in_=w2, func=AF.Identity, bias=cst[:, 1:2])], reads=['cst'], writes=['w2', 'w1'])
            r.op('dve', [lambda e: e.reciprocal(out=w1, in_=w1)], writes=['w1'])
            r.op('dve', [lambda e: e.tensor_tensor(out=w2, in0=w2, in1=w1, op=ALU.mult)], writes=['w1', 'w2'])
            r.op('dve', [lambda e: e.tensor_tensor(out=eq1, in0=eq1, in1=bc(w1), op=ALU.mult)], writes=['eq1', 'w1'])
            r.op('dve', [lambda e: e.tensor_tensor(out=eq2, in0=eq2, in1=bc(w2), op=ALU.mult)], writes=['eq2', 'w2'])
            r.op('dve', [lambda e: e.tensor_tensor(out=gt, in0=eq1, in1=eq2, op=ALU.add)], writes=['eq1', 'eq2', 'gt'])
            for q in range(4):
                r.op('pe', [lambda e, t=t, q=q: e.transpose(out=bank(q, 128, (t % 4) * 128)[0:8, :], in_=gt[:, t, :], identity=ident[:]) for t in range(q * 4, q * 4 + 4)],
                     reads=['gt', 'ident'], writes=[PB(q)])
                r.op('dve', [lambda e, q=q: e.tensor_copy(out=gatesT[0:8, q * 512:(q + 1) * 512], in_=bank(q)[0:8, :])], writes=[PB(q), 'gatesT'])

        def moe_gates_sparse(cvm, logitsT, posmb, gtp, flags_i, iota):
            def t3(n=128):
                return cvm.f32(n).rearrange("p (t e) -> p t e", e=8)
            lg, l2, eq1, eq2, msk, pref, tot, off, pos = (t3() for _ in range(9))
            m1, m2, w1, w2 = (cvm.f32(16) for _ in range(4))
            ne = cvm.f32(8)
            flagsf = cvm.f32(32).rearrange("p (e b) -> p e b", b=4)
            ustr = cvm.f32(128)
            ones1 = cvm.f32(128)
            AX = mybir.AxisListType.X

            def bc(v):
                return mkap(v[:, 0:1], [[1, 16], [0, 8]])

            def fl(v):
                return v.rearrange("p t e -> p (t e)")

            r.op('sp', [lambda e: e.dma_start(out=ustr, in_=ustr_d)], writes=['ustr'], dma='c5')
            r.op('sp', [lambda e: e.dma_start(out=iota, in_=iota_d)], writes=['iota'], dma='c6')
            r.op('pool', [lambda e: e.memset(ones1, 1.0)], writes=['ones1'])
            r.op('pe', [lambda e, t=t: e.transpose(out=bank(0, 8, t * 8), in_=logitsT[0:8, t * 128:(t + 1) * 128], identity=ident[0:8, 0:8]) for t in range(16)],
                 reads=['logitsT', 'ident'], writes=[PB(0)])
            D = 'gsp'
            r.op('dve', [lambda e: e.tensor_copy(out=lg, in_=bank(0, 128).rearrange("p (t e) -> p t e", e=8))], writes=[PB(0), D])
            r.op('dve', [lambda e: e.tensor_reduce(out=m1, in_=lg, axis=AX, op=ALU.max)], writes=[D])
            r.op('dve', [lambda e: e.tensor_tensor(out=eq1, in0=lg, in1=bc(m1), op=ALU.is_equal)], writes=[D])
            r.op('dve', [lambda e: e.scalar_tensor_tensor(out=l2, in0=eq1, scalar=cst[:, 2:3], in1=lg, op0=ALU.mult, op1=ALU.add)], reads=['cst'], writes=[D])
            r.op('dve', [lambda e: e.tensor_reduce(out=m2, in_=l2, axis=AX, op=ALU.max)], writes=[D])
            r.op('dve', [lambda e: e.tensor_tensor(out=eq2, in0=l2, in1=bc(m2), op=ALU.is_equal)], writes=[D])
            r.op('dve', [lambda e: e.tensor_tensor(out=msk, in0=eq1, in1=eq2, op=ALU.add)], writes=[D])
            r.op('dve', [lambda e: e.tensor_tensor(out=w2, in0=m2, in1=m1, op=ALU.subtract)], writes=[D])
            r.op('act', [lambda e: e.activation(out=w2, in_=w2, func=AF.Exp)], writes=[D])
            r.op('act', [lambda e: e.activation(out=w1, in_=w2, func=AF.Identity, bias=cst[:, 1:2])], reads=['cst'], writes=[D])
            r.op('dve', [lambda e: e.reciprocal(out=w1, in_=w1)], writes=[D])
            r.op('dve', [lambda e: e.tensor_tensor(out=w2, in0=w2, in1=w1, op=ALU.mult)], writes=[D])
            r.op('dve', [lambda e: e.tensor_tensor(out=eq1, in0=eq1, in1=bc(w1), op=ALU.mult)], writes=[D])
            r.op('dve', [lambda e: e.tensor_tensor(out=eq2, in0=eq2, in1=bc(w2), op=ALU.mult)], writes=[D])
            r.op('dve', [lambda e: e.tensor_tensor(out=gtp, in0=eq1, in1=eq2, op=ALU.add)], writes=[D, 'gtp'])
            r.op('pe', [lambda e: e.matmul(bank(1, 128), ustr, fl(msk), start=True, stop=True)], reads=[D, 'ustr'], writes=[PB(1)])
            r.op('pe', [lambda e: e.matmul(bank(2, 128), ones1, fl(msk), start=True, stop=True)], reads=[D, 'ones1'], writes=[PB(2)])
            r.op('dve', [lambda e: e.tensor_copy(out=fl(pref), in_=bank(1, 128))], writes=[PB(1), D])
            r.op('dve', [lambda e: e.tensor_copy(out=fl(tot), in_=bank(2, 128))], writes=[PB(2), D])
            r.op('dve', [lambda e: e.memset(off[:, 0, :], 0.0)], writes=[D])
            for t in range(1, 16):
                r.op('dve', [lambda e, t=t: e.tensor_tensor(out=off[:, t, :], in0=off[:, t - 1, :], in1=tot[:, t - 1, :], op=ALU.add)], writes=[D])
            r.op('dve', [lambda e: e.tensor_tensor(out=ne, in0=off[:, 15, :], in1=tot[:, 15, :], op=ALU.add)], writes=[D])
            r.op('dve', [lambda e: e.tensor_tensor(out=pos, in0=pref, in1=off, op=ALU.add)], writes=[D])
            r.op('dve', [lambda e: e.scalar_tensor_tensor(out=pos, in0=pos, scalar=cst[:, 1:2], in1=msk, op0=ALU.add, op1=ALU.mult)], reads=['cst'], writes=[D])
            for b_ in range(4):
                r.op('dve', [lambda e, b_=b_: e.tensor_scalar(out=posmb[:, b_, :, :], in0=pos, scalar1=cst[:, 4:5], scalar2=cst[:, 9 + b_:10 + b_], op0=ALU.add, op1=ALU.add)],
                     reads=['cst'], writes=[D, 'posmb'])
                r.op('dve', [lambda e, b_=b_: e.tensor_scalar(out=flagsf[:, :, b_], in0=ne, scalar1=cst[:, 5 + b_:6 + b_], scalar2=cst[:, 1:2], op0=ALU.is_gt, op1=ALU.mult)],
                     reads=['cst'], writes=[D])
            r.op('dve', [lambda e: e.tensor_copy(out=flags_i[:], in_=flagsf.rearrange("p e b -> p (e b)"))], writes=[D, 'flags'])

        def moe_sparse_phase(l, cvm, posmb, gtp, flags_i, iota):
            aTf = aT[:].rearrange("p c t -> p (c t)")
            W1 = [aTf[:, i * 6144:i * 6144 + 2048].rearrange("p (k n) -> p k n", k=8) for i in range(2)]
            W3 = [aTf[:, i * 6144 + 2048:i * 6144 + 4096].rearrange("p (k n) -> p k n", k=8) for i in range(2)]
            W2 = [aTf[:, i * 6144 + 4096:i * 6144 + 6144].rearrange("p (f n) -> p f n", f=2) for i in range(2)]
            G = [aTf[:, 12288 + i * 1024:12288 + (i + 1) * 1024].rearrange("p (f n) -> p f n", f=2) for i in range(2)]
            a2g = aTf[:, 14336:18432].rearrange("p (c n) -> p c n", c=8)
            Selb = [cvm.bf16(512) for _ in range(4)]
            SelT = [cvm.bf16(2048) for _ in range(2)]
            outS = cvm.f32(4096).rearrange("p (s n) -> p s n", s=4)
            outSb = cvm.bf16(4096).rearrange("p (s n) -> p s n", s=4)
            S = [cvm.f32(512) for _ in range(2)]
            psTb = ps[:, 0:1024].bitcast(BF16)
            cn = {'sel': 0, 'w': 0, 'h': 0, 'g': 0, 'st': 0}
            NGRP = DFF // 256
            for e_ in range(8):
                for b_ in range(4):
                    kf = e_ * 4 + b_
                    if b_ in (1, 2):
                        r.begin_region(flags_i[0:1, kf:kf + 1], 'flags')
                    for t in range(16):
                        si = cn['sel'] % 4
                        cn['sel'] += 1
                        sbuf_ = Selb[si]
                        r.op('dve', [lambda e, sbuf_=sbuf_, t=t, b_=b_, e_=e_: e.tensor_scalar(
                            out=sbuf_, in0=iota, scalar1=posmb[:, b_, t, e_:e_ + 1], scalar2=cst[:, 1:2], op0=ALU.is_equal, op1=ALU.mult)],
                            reads=['posmb', 'iota', 'cst'], writes=[f'Sel{si}'])
                        r.op('pe', [lambda e, c=c, t=t, sbuf_=sbuf_: e.matmul(bank(c), a2tok(t)[:, c * 128:(c + 1) * 128], sbuf_, start=(t == 0), stop=(t == 15)) for c in range(8)],
                             reads=[f'Sel{si}', f'a2tok{t}'], writes=[PB(c) for c in range(8)])
                    for c in range(8):
                        copy_any(a2g[:, c, :], bank(c), [], [PB(c), f'a2g{c}'])
                    for g in range(NGRP):
                        wb = cn['w'] % 2
                        cn['w'] += 1
                        w1s = moe_w1[0, e_][:, g * 256:(g + 1) * 256].rearrange("(k p) n -> p k n", p=128)
                        w3s = moe_w3[0, e_][:, g * 256:(g + 1) * 256].rearrange("(k p) n -> p k n", p=128)
                        w2s = moe_w2[0, e_][g * 256:(g + 1) * 256, :].rearrange("(f p) n -> p f n", p=128)
                        r.op('pool', [lambda e, wb=wb, w1s=w1s: e.dma_start(out=W1[wb], in_=w1s)], writes=[f'W1{wb}'], dma=f'W1{wb}')
                        r.op('pool', [lambda e, wb=wb, w3s=w3s: e.dma_start(out=W3[wb], in_=w3s)], writes=[f'W3{wb}'], dma=f'W3{wb}')
                        r.op('pool', [lambda e, wb=wb, w2s=w2s: e.dma_start(out=W2[wb], in_=w2s)], writes=[f'W2{wb}'], dma=f'W2{wb}')
                        gi = cn['g'] % 2
                        cn['g'] += 1
                        for fc in range(2):
                            k2 = cn['h'] % 2
                            cn['h'] += 1
                            p1, p3 = k2, 2 + k2
                            r.op('pe', [lambda e, kk=kk, p1=p1, fc=fc, wb=wb: e.matmul(bank(p1), W1[wb][:, kk, fc * 128:(fc + 1) * 128], a2g[:, kk, :],
                                                                                      start=(kk == 0), stop=(kk == 7)) for kk in range(8)],
                                 reads=[f'W1{wb}'] + [f'a2g{kk}' for kk in range(8)], writes=[PB(p1)])
                            r.op('pe', [lambda e, kk=kk, p3=p3, fc=fc, wb=wb: e.matmul(bank(p3), W3[wb][:, kk, fc * 128:(fc + 1) * 128], a2g[:, kk, :],
                                                                                      start=(kk == 0), stop=(kk == 7)) for kk in range(8)],
                                 reads=[f'W3{wb}'] + [f'a2g{kk}' for kk in range(8)], writes=[PB(p3)])
                            sk = S[k2]
                            r.op('act', [lambda e, p1=p1, sk=sk: e.activation(out=sk, in_=bank(p1), func=AF.Silu)], writes=[PB(p1), f'S{k2}'])
                            r.op('dve', [lambda e, p3=p3, sk=sk, fc=fc, gi=gi: e.tensor_tensor(out=G[gi][:, fc, :], in0=bank(p3), in1=sk, op=ALU.mult)],
                                 reads=[f'S{k2}'], writes=[PB(p3), f'G{gi}'])
                        for s_ in range(4):
                            for dh in range(2):
                                pb = 4 + (s_ % 2) * 2 + dh
                                r.op('pe', [lambda e, fc=fc, pb=pb, s_=s_, dh=dh, gi=gi, wb=wb: e.matmul(
                                    bank(pb), G[gi][:, fc, s_ * 128:(s_ + 1) * 128], W2[wb][:, fc, dh * 512:(dh + 1) * 512], start=(fc == 0), stop=(fc == 1)) for fc in range(2)],
                                    reads=[f'G{gi}', f'W2{wb}'], writes=[PB(pb)])
                                dst = outS[:, s_, dh * 512:(dh + 1) * 512]
                                if g == 0:
                                    r.op('dve', [lambda e, pb=pb, dst=dst: e.tensor_copy(out=dst, in_=bank(pb))], writes=[PB(pb), f'outS{s_}'])
                                else:
                                    r.op('dve', [lambda e, pb=pb, dst=dst: e.tensor_tensor(out=dst, in0=bank(pb), in1=dst, op=ALU.add)], writes=[PB(pb), f'outS{s_}'])
                    for s_ in range(4):
                        copy_any(outSb[:, s_, :], outS[:, s_, :], [f'outS{s_}'], [f'outSb{s_}'])
                    for tb in range(4):
                        kt = cn['st'] % 2
                        cn['st'] += 1
                        for tt in range(4):
                            t = tb * 4 + tt
                            si = cn['sel'] % 4
                            cn['sel'] += 1
                            sbuf_ = Selb[si]
                            r.op('dve', [lambda e, sbuf_=sbuf_, t=t, b_=b_, e_=e_: e.tensor_scalar(
                                out=sbuf_, in0=iota, scalar1=posmb[:, b_, t, e_:e_ + 1], scalar2=gtp[:, t, e_:e_ + 1], op0=ALU.is_equal, op1=ALU.mult)],
                                reads=['posmb', 'iota', 'gtp'], writes=[f'Sel{si}'])
                            r.op('pe', [lambda e, s_=s_, tt=tt, sbuf_=sbuf_: e.transpose(out=psTb[:, s_ * 512 + tt * 128:s_ * 512 + (tt + 1) * 128],
                                                                                         in_=sbuf_[:, s_ * 128:(s_ + 1) * 128], identity=identb[:]) for s_ in range(4)],
                                 reads=[f'Sel{si}', 'identb'], writes=[PB(0), PB(1)])
                        copy_any(SelT[kt], psTb, [], [PB(0), PB(1), f'SelT{kt}'])
                        stv = SelT[kt].rearrange("p (s n) -> p s n", s=4)
                        for c in range(8):
                            pb = 4 + c % 4
                            r.op('pe', [lambda e, s_=s_, c=c, pb=pb, stv=stv: e.matmul(bank(pb), outSb[:, s_, c * 128:(c + 1) * 128], stv[:, s_, :],
                                                                                      start=(s_ == 0), stop=(s_ == 3)) for s_ in range(4)],
                                 reads=[f'outSb{s_}' for s_ in range(4)] + [f'SelT{kt}'], writes=[PB(pb)])
                            g2 = mod[:, l, 40 + c, 0:1]
                            r.op('dve', [lambda e, pb=pb, c=c, tb=tb, g2=g2: e.scalar_tensor_tensor(
                                out=hT[:, c, tb * 512:(tb + 1) * 512], in0=bank(pb), scalar=g2, in1=hT[:, c, tb * 512:(tb + 1) * 512],
                                op0=ALU.mult, op1=ALU.add)], reads=[f'mod{l}'], writes=[PB(pb), f'hT:{c}:{tb}'])
                    if b_ in (1, 3):
                        r.end_region()

        def final_phase():
            cvf = Carver()
            sq = [cvf.f32(512) for _ in range(2)]
            lnv = cvf.f32(512)
            rs = cvf.f32(512)
            tmA = cvf.f32(8 * 512).rearrange("p (c n) -> p c n", c=8)
            ost = [cvf.f32(1024) for _ in range(2)]
            for bi in range(4):
                t0, sz = BLKS[bi]
                for c in range(8):
                    s = sq[c % 2]
                    r.op('act', [lambda e, s=s, c=c, t0=t0, sz=sz: e.activation(out=s, in_=hT[:, c, t0:t0 + sz], func=AF.Square)], reads=[f'hT:{c}:{bi}'], writes=[f'sq{c % 2}'])
                    r.op('pe', [lambda e, s=s, c=c: e.matmul(bank(0), onesf[:], s, start=(c == 0), stop=(c == 7))], reads=[f'sq{c % 2}', 'onesf'], writes=[PB(0)])
                r.op('act', [lambda e: e.activation(out=lnv, in_=bank(0), func=AF.Ln, bias=cst[:, 0:1])], reads=['cst'], writes=[PB(0), 'lnv'])
                r.op('act', [lambda e: e.activation(out=rs, in_=lnv, func=AF.Exp, scale=-0.5)], reads=['lnv'], writes=['rs'])
                for c in range(8):
                    r.op('dve', [lambda e, c=c, t0=t0, sz=sz: e.scalar_tensor_tensor(out=tmA[:, c, :], in0=hT[:, c, t0:t0 + sz], scalar=vcol(R_FINALG + c), in1=rs,
                                                                                     op0=ALU.mult, op1=ALU.mult)], reads=[f'hT:{c}:{bi}', 'rs', 'vT'], writes=[f'tmA{c}'])
                for tt in range(4):
                    tile = bi * 4 + tt
                    k2 = tile % 2
                    for half in range(2):
                        pb = 2 + k2 * 2 + half
                        r.op('pe', [lambda e, c=c, pb=pb, tt=tt: e.transpose(out=bank(pb, 128, (c % 4) * 128), in_=tmA[:, c, tt * 128:(tt + 1) * 128], identity=ident[:])
                                    for c in range(half * 4, half * 4 + 4)], reads=[f'tmA{c}' for c in range(half * 4, half * 4 + 4)] + ['ident'], writes=[PB(pb)])
                        copy_any(ost[k2][:, half * 512:(half + 1) * 512], bank(pb), [], [PB(pb), f'ost{k2}'])
                    r.op('sp', [lambda e, k2=k2, tile=tile: e.dma_start(out=out_d[tile * 128:(tile + 1) * 128, :], in_=ost[k2])],
                         reads=[f'ost{k2}'], writes=[f'out{tile}'], dma=f'out{k2}')
            r.op('sp', [], reads=[f'out{t}' for t in range(16)], final=True)

        adaln(0)
        adaln(1)
        r.barrier()
        done = False
        if debug == 'load':
            dump_hT_and_finish()
            done = True
        for l in range(2):
            if done:
                break
            last = (l == 1)
            norm_mod(l, 0, range(5), Carver())
            if debug == f'a1_{l}':
                dump_bf16(aT, 8, lambda c: []); done = True; break
            r.barrier()
            lru_phase(l, last)
            if debug == f'lru_{l}':
                dump_bf16(gro, 4, lambda c: []); done = True; break
            r.barrier()
            attn_phase(l, last)
            if debug == f'attn_{l}':
                dump_bf16(qo, 4, lambda c: []); done = True; break
            r.barrier()
            wout_phase(l, last)
            if debug == f'wout_{l}':
                dump_hT_and_finish(); done = True; break
            if not last:
                cvn2 = Carver()
                cvn2.off = 4096
                norm_mod(l, 1, range(5), cvn2)
                r.barrier()
                ffn_phase(l, False, range(5), Carver())
            else:
                r.barrier()
                cvm = Carver()
                if MOE_SPARSE:
                    posmb = cvm.f32(512).rearrange("p (b t e) -> p b t e", b=4, t=16)
                    gtp = cvm.f32(128).rearrange("p (t e) -> p t e", e=8)
                    iota = cvm.f32(512)
                    keep = cvm.off
                    logitsT = cvm.f32(NL)
                    norm_mod(l, 1, range(4), cvm, router=True, logitsT=logitsT)
                    moe_gates_sparse(cvm, logitsT, posmb, gtp, flags_i, iota)
                    r.barrier()
                    cvm.off = keep
                    moe_sparse_phase(l, cvm, posmb, gtp, flags_i, iota)
                else:
                    gatesT = cvm.f32(NL)
                    logitsT = cvm.f32(NL)
                    norm_mod(l, 1, range(4), cvm, router=True, logitsT=logitsT)
                    moe_gates(cvm, logitsT, gatesT)
                    r.barrier()
                    cvm.off = NL
                    ffn_phase(l, True, range(4), cvm, gatesT=gatesT)
            if debug == f'ffn_{l}':
                dump_hT_and_finish(); done = True; break
            r.barrier()
        if not done:
            final_phase()
        emit(nc, r, es)
    return nc


def _host_tables(inp):
    f32 = np.float32
    kc = np.arange(64)[:, None]
    qc = np.arange(64)[None, :]
    dcm = np.clip(kc - qc, -15, 15) + 15
    cs = np.clip(qc - 8, 0, 48)
    ok = (kc >= cs) & (kc < cs + 16)
    tb = np.full((2, 4, 2, 2, 128, 16, 64), NEG, f32)
    rpb = np.asarray(inp['na_rpb'], f32)
    for l in range(2):
        for h in range(8):
            hp, j = divmod(h, 2)
            for half in range(2):
                for idx in range(16):
                    d = (7 - idx) if half == 0 else (8 - idx)
                    if -7 <= d <= 7:
                        vals = rpb[l, h, d + 7][dcm]
                        tb[l, hp, 0, j, half * 64:(half + 1) * 64, idx, :] = np.where(ok, vals, f32(NEG))
                        if -4 <= d <= 3:
                            tb[l, hp, 1, j, half * 64:(half + 1) * 64, idx, :] = np.where(ok, vals, f32(NEG))
    tb = tb.reshape(2, 4, 2, 2, 128, 1024)
    wbd = np.zeros((2, 2, 2, 4, 128, 128), f32)
    for a, nm in enumerate(('lru_wa', 'lru_wx')):
        w = np.asarray(inp[nm], f32)
        for c in range(4):
            for s in range(2):
                wbd[:, :, a, c, s * 64:(s + 1) * 64, s * 64:(s + 1) * 64] = w[:, :, c * 2 + s]
    ident = np.eye(128, dtype=f32)
    sel = np.zeros((8, 8, 128), f32)
    for e in range(8):
        sel[e, e, :] = 1.0
    sel = sel.reshape(8, 1024)
    return tb, wbd, ident, sel


def _consts():
    f32 = np.float32
    k = np.arange(128)
    ustrict = (k[:, None] < k[None, :]).astype(f32)
    iota = np.ascontiguousarray(np.broadcast_to(np.arange(512, dtype=f32)[None, :], (128, 512)))
    return ustrict, iota


def _vecs(inp, b):
    f32 = np.float32
    rows = []
    for l in range(2):
        rows.append(np.asarray(inp['ada_b'][l], f32).reshape(48, 128))
        rows.append(np.asarray(inp['mix_norm_g'][l], f32).reshape(8, 128))
        rows.append(np.asarray(inp['ffn_norm_g'][l], f32).reshape(8, 128))
        rows.append(np.asarray(inp['conv_w'][l], f32).reshape(16, 128))
        rows.append(np.asarray(inp['conv_b'][l], f32).reshape(4, 128))
        rows.append(np.asarray(inp['lru_ba'][l], f32).reshape(8, 128))
        rows.append(np.asarray(inp['lru_bx'][l], f32).reshape(8, 128))
        rows.append(np.asarray(inp['lru_lam'][l], f32).reshape(8, 128))
    rows.append(np.asarray(inp['final_g'], f32).reshape(8, 128))
    rows.append(np.asarray(inp['c'][b], f32).reshape(8, 128))
    rows.append(np.asarray(inp['c_ctx'], f32).reshape(8, 128))
    v = np.concatenate(rows, 0)
    out = np.zeros((256, 128), f32)
    out[:v.shape[0]] = v
    return out


_NC_CACHE = {}


def make_in_maps(inp, cores):
    tb, wbd, ident, sel = _host_tables(inp)
    f32 = np.float32
    shared = {
        'ada_w': np.ascontiguousarray(inp['ada_w'], f32), 'w_in': np.ascontiguousarray(inp['w_in'], f32),
        'w_out': np.ascontiguousarray(inp['w_out'], f32),
        'ffn_w1': np.ascontiguousarray(inp['ffn_w1'], f32), 'ffn_w3': np.ascontiguousarray(inp['ffn_w3'], f32),
        'ffn_w2': np.ascontiguousarray(inp['ffn_w2'], f32),
        'moe_w1': np.ascontiguousarray(inp['moe_w1'], f32), 'moe_w3': np.ascontiguousarray(inp['moe_w3'], f32),
        'moe_w2': np.ascontiguousarray(inp['moe_w2'], f32),
        'moe_router': np.ascontiguousarray(inp['moe_router'], f32),
        'rb': np.ascontiguousarray(np.asarray(inp['moe_router_b'], f32).reshape(8, 1)),
        'wbd': wbd, 'tb': tb, 'ident': ident, 'sel': sel,
        'ustrict': _consts()[0], 'iota': _consts()[1],
    }
    maps = []
    for b in cores:
        m = dict(shared)
        m['x'] = np.ascontiguousarray(inp['x'][b], f32)
        m['ctx'] = np.ascontiguousarray(inp['ctx'][b], f32)
        m['vecs'] = _vecs(inp, b)
        maps.append(m)
    return maps


def kernel(**inputs):
    inp = {k: np.asarray(v) for k, v in inputs.items()}
    if 'nc' not in _NC_CACHE:
        _NC_CACHE['nc'] = build_program()
    nc = _NC_CACHE['nc']
    maps = make_in_maps(inp, range(8))
    res = run_bass_kernel_spmd(nc, maps, core_ids=list(range(8)))
    out = np.stack([np.asarray(res.results[b]['out'], np.float32) for b in range(8)], 0)
    return out
```
